# Optimizing a Trainium2 kernel written in Bass

```python
import math
import jax, jax.numpy as jnp
from jax import lax
import numpy as np

D_MODEL = 1024
BATCH = 8
SEQ = 2048
DEPTH = 1

HEAD_DIM = 64
SCALE = HEAD_DIM ** -0.5
Q_BLOCK = 128
NSA_HEADS = 8
NSA_KV_GROUPS = 2
NSA_HPG = NSA_HEADS // NSA_KV_GROUPS
CMP_BLOCK = 32
CMP_STRIDE = 16
CMP_HIDDEN = 256
SLC_BLOCK = 64
SLC_TOP_N = 16
SLC_Q_CHUNK = 32
WINDOW = 512
FORCE_SCORE = 1e4
DIFF_HEADS = 4
DIFF_V_DIM = 2 * HEAD_DIM
REL_BUCKETS = 32
REL_MAX_DIST = 128
N_ATTN_HEADS = NSA_HEADS + DIFF_HEADS
N_GROUPS = 4
EXPERTS_PER_GROUP = 4
N_EXPERTS = N_GROUPS * EXPERTS_PER_GROUP
TOP_K_IN_GROUP = 2
EXPERT_HIDDEN = 512
COL_NSA_Q = NSA_HEADS * HEAD_DIM
COL_NSA_KV = 3 * 2 * NSA_KV_GROUPS * HEAD_DIM
COL_NSA_GATE = 3 * NSA_HEADS
COL_DIFF_Q = DIFF_HEADS * 2 * HEAD_DIM
COL_DIFF_K = DIFF_HEADS * 2 * HEAD_DIM
COL_DIFF_V = DIFF_HEADS * DIFF_V_DIM
COL_MERGE = 2 * D_MODEL
IN_COLS = COL_NSA_Q + COL_NSA_KV + COL_NSA_GATE + COL_DIFF_Q + COL_DIFF_K + COL_DIFF_V + COL_MERGE
RMS_EPS = 1e-6
NEG = -1e30

kernel_name = "hybrid_nsa_diffattn_hmoe_block"


def rms_norm(x, g):
    xf = x.astype(jnp.float32)
    y = xf * lax.rsqrt(jnp.mean(xf * xf, axis=-1, keepdims=True) + RMS_EPS)
    return (y * g.astype(jnp.float32)).astype(x.dtype)


def masked_softmax(logits, mask):
    logits = jnp.where(mask, logits.astype(jnp.float32), NEG)
    p = jax.nn.softmax(logits, axis=-1)
    return p * jnp.any(mask, axis=-1, keepdims=True)


def t5_bucket(dist):
    n = jnp.maximum(dist, 0)
    max_exact = REL_BUCKETS // 2
    nf = jnp.maximum(n, 1).astype(jnp.float32)
    large = max_exact + (jnp.log(nf / max_exact) / math.log(REL_MAX_DIST / max_exact)
                         * (REL_BUCKETS - max_exact)).astype(jnp.int32)
    large = jnp.minimum(large, REL_BUCKETS - 1)
    return jnp.where(n < max_exact, n, large)


def nsa_compress(tok, pos_emb, w1, w2):
    B, S, G, dh = tok.shape
    n_cmp = (S - CMP_BLOCK) // CMP_STRIDE + 1
    idx = np.arange(n_cmp)[:, None] * CMP_STRIDE + np.arange(CMP_BLOCK)[None, :]
    blocks = tok[:, idx] + pos_emb[:, None, :]
    blocks = blocks.transpose(0, 1, 3, 2, 4).reshape(B, n_cmp, G, CMP_BLOCK * dh)
    return jax.nn.gelu(blocks @ w1) @ w2


def nsa_compressed_attention(q, k_cmp, v_cmp, tbl):
    S = q.shape[1]
    n_cmp = k_cmp.shape[1]
    block_end = jnp.arange(n_cmp) * CMP_STRIDE + CMP_BLOCK - 1
    dist = jnp.arange(S)[:, None] - block_end[None, :]
    mask = dist >= 0
    bias = tbl[:, :, t5_bucket(dist)]
    logits = jnp.einsum('bsghd,bngd->bghsn', q, k_cmp) * SCALE + bias
    p = masked_softmax(logits, mask)
    o = jnp.einsum('bghsn,bngd->bsghd', p.astype(v_cmp.dtype), v_cmp)
    return o, p


def nsa_select_blocks(p_cmp):
    S, n_cmp = p_cmp.shape[3], p_cmp.shape[4]
    n_slc = S // SLC_BLOCK
    c_start = np.arange(n_cmp) * CMP_STRIDE
    s_start = np.arange(n_slc) * SLC_BLOCK
    lo = np.maximum(s_start[:, None], c_start[None, :])
    hi = np.minimum(s_start[:, None] + SLC_BLOCK, c_start[None, :] + CMP_BLOCK)
    overlap = (np.maximum(hi - lo, 0) / CMP_BLOCK).astype(np.float32)
    imp = jnp.einsum('bghsn,jn->bgsj', p_cmp, jnp.asarray(overlap))
    blk = jnp.arange(n_slc)[None, :]
    cur = (jnp.arange(S) // SLC_BLOCK)[:, None]
    valid = blk <= cur
    forced = (blk == 0) | (blk == cur) | (blk == cur - 1)
    score = jnp.where(valid, imp + jnp.where(forced, FORCE_SCORE, 0.0), NEG)
    _, idx = lax.top_k(score, min(SLC_TOP_N, n_slc))
    return idx


def nsa_selected_attention(q, k_slc, v_slc, sel_idx, tbl):
    B, S, G, Hg, dh = q.shape
    n_slc = S // SLC_BLOCK
    k_blocks = k_slc.reshape(B, n_slc, SLC_BLOCK, G, dh).transpose(0, 3, 1, 2, 4)
    v_blocks = v_slc.reshape(B, n_slc, SLC_BLOCK, G, dh).transpose(0, 3, 1, 2, 4)
    q_g = q.transpose(0, 2, 3, 1, 4)
    b_ix = jnp.arange(B)[:, None, None, None]
    g_ix = jnp.arange(G)[None, :, None, None]
    tg = jnp.arange(G)[None, :, None, None, None, None]
    th = jnp.arange(Hg)[None, None, :, None, None, None]
    in_block = jnp.arange(SLC_BLOCK)
    n_keys = sel_idx.shape[-1] * SLC_BLOCK

    def chunk(c):
        s0 = c * SLC_Q_CHUNK
        qc = lax.dynamic_slice_in_dim(q_g, s0, SLC_Q_CHUNK, axis=3)
        ic = lax.dynamic_slice_in_dim(sel_idx, s0, SLC_Q_CHUNK, axis=2)
        kc = k_blocks[b_ix, g_ix, ic]
        vc = v_blocks[b_ix, g_ix, ic]
        tq = s0 + jnp.arange(SLC_Q_CHUNK)
        dist = tq[None, None, :, None, None] - (ic[..., None] * SLC_BLOCK + in_block)
        bias = tbl[tg, th, t5_bucket(dist)[:, :, None]]
        logits = jnp.einsum('bghqd,bgqnld->bghqnl', qc, kc) * SCALE + bias
        p = masked_softmax(logits.reshape(B, G, Hg, SLC_Q_CHUNK, n_keys),
                           (dist >= 0).reshape(B, G, 1, SLC_Q_CHUNK, n_keys))
        return jnp.einsum('bghqm,bgqmd->bghqd', p.astype(vc.dtype),
                          vc.reshape(B, G, SLC_Q_CHUNK, n_keys, dh))

    out = lax.map(chunk, jnp.arange(S // SLC_Q_CHUNK))
    return out.transpose(1, 0, 4, 2, 3, 5).reshape(B, S, G, Hg, dh)


def nsa_window_attention(q, k_win, v_win, tbl):
    B, S, G, Hg, dh = q.shape
    q_g = q.transpose(0, 2, 3, 1, 4)
    pad = ((0, 0), (WINDOW, 0), (0, 0), (0, 0))
    kp = jnp.pad(k_win, pad)
    vp = jnp.pad(v_win, pad)
    span = WINDOW + Q_BLOCK
    r = jnp.arange(span)
    dist = jnp.arange(Q_BLOCK)[:, None] - (r[None, :] - WINDOW)
    in_band = (dist >= 0) & (dist < WINDOW)
    bias = tbl[:, :, t5_bucket(dist)]

    def block(i):
        s0 = i * Q_BLOCK
        qb = lax.dynamic_slice_in_dim(q_g, s0, Q_BLOCK, axis=3)
        kb = lax.dynamic_slice_in_dim(kp, s0, span, axis=1)
        vb = lax.dynamic_slice_in_dim(vp, s0, span, axis=1)
        mask = in_band & (s0 - WINDOW + r >= 0)[None, :]
        logits = jnp.einsum('bghqd,bkgd->bghqk', qb, kb) * SCALE + bias
        p = masked_softmax(logits, mask)
        return jnp.einsum('bghqk,bkgd->bghqd', p.astype(vb.dtype), vb)

    out = lax.map(block, jnp.arange(S // Q_BLOCK))
    return out.transpose(1, 0, 4, 2, 3, 5).reshape(B, S, G, Hg, dh)


def diff_attention(q1, q2, k1, k2, v, lam, tbl):
    B, S, H, _ = q1.shape
    kpos = jnp.arange(S)

    def block(i):
        s0 = i * Q_BLOCK
        dist = (s0 + jnp.arange(Q_BLOCK))[:, None] - kpos[None, :]
        mask = dist >= 0
        bias = tbl[:, t5_bucket(dist)]
        q1b = lax.dynamic_slice_in_dim(q1, s0, Q_BLOCK, axis=1)
        q2b = lax.dynamic_slice_in_dim(q2, s0, Q_BLOCK, axis=1)
        a1 = masked_softmax(jnp.einsum('bqhd,bkhd->bhqk', q1b, k1) * SCALE + bias, mask)
        a2 = masked_softmax(jnp.einsum('bqhd,bkhd->bhqk', q2b, k2) * SCALE + bias, mask)
        attn = a1 - lam * a2
        return jnp.einsum('bhqk,bkhe->bqhe', attn.astype(v.dtype), v)

    out = lax.map(block, jnp.arange(S // Q_BLOCK))
    return out.transpose(1, 0, 2, 3, 4).reshape(B, S, H, v.shape[-1])


def mixer_layer(h, w_in, nsa_q_norm, nsa_k_norm, cmp_pos, cmp_w1, cmp_w2,
                diff_q_norm, diff_k_norm, diff_lambda, diff_out_norm, rel_bias,
                w_branch_nsa, w_branch_diff, w_out, lambda_init):
    B, S, _ = h.shape
    G, Hg, dh = NSA_KV_GROUPS, NSA_HPG, HEAD_DIM
    proj = h @ w_in
    sizes = (COL_NSA_Q, COL_NSA_KV, COL_NSA_GATE, COL_DIFF_Q, COL_DIFF_K, COL_DIFF_V)
    points, acc = [], 0
    for s in sizes:
        acc += s
        points.append(acc)
    q_nsa, kv_nsa, g_nsa, q_diff, k_diff, v_diff, g_merge = jnp.split(proj, points, axis=-1)

    tbl_nsa = rel_bias[:, :NSA_HEADS].T.reshape(G, Hg, REL_BUCKETS)
    tbl_diff = rel_bias[:, NSA_HEADS:].T

    q = rms_norm(q_nsa.reshape(B, S, G, Hg, dh), nsa_q_norm)
    kv = kv_nsa.reshape(B, S, 3, 2, G, dh)
    k_cmp = rms_norm(nsa_compress(kv[:, :, 0, 0], cmp_pos[0], cmp_w1[0], cmp_w2[0]), nsa_k_norm[0])
    v_cmp = nsa_compress(kv[:, :, 0, 1], cmp_pos[1], cmp_w1[1], cmp_w2[1])
    k_slc = rms_norm(kv[:, :, 1, 0], nsa_k_norm[1])
    v_slc = kv[:, :, 1, 1]
    k_win = rms_norm(kv[:, :, 2, 0], nsa_k_norm[2])
    v_win = kv[:, :, 2, 1]
    o_cmp, p_cmp = nsa_compressed_attention(q, k_cmp, v_cmp, tbl_nsa)
    sel_idx = nsa_select_blocks(p_cmp)
    o_slc = nsa_selected_attention(q, k_slc, v_slc, sel_idx, tbl_nsa)
    o_win = nsa_window_attention(q, k_win, v_win, tbl_nsa)
    gates = jax.nn.sigmoid(g_nsa.reshape(B, S, 3, G, Hg, 1))
    o_nsa = gates[:, :, 0] * o_cmp + gates[:, :, 1] * o_slc + gates[:, :, 2] * o_win
    o_nsa = o_nsa.reshape(B, S, NSA_HEADS * dh)

    qd = rms_norm(q_diff.reshape(B, S, DIFF_HEADS, 2, dh), diff_q_norm)
    kd = rms_norm(k_diff.reshape(B, S, DIFF_HEADS, 2, dh), diff_k_norm)
    vd = v_diff.reshape(B, S, DIFF_HEADS, DIFF_V_DIM)
    lam_f = diff_lambda.astype(jnp.float32)
    lam = (jnp.exp(jnp.sum(lam_f[0] * lam_f[1])) - jnp.exp(jnp.sum(lam_f[2] * lam_f[3]))
           + lambda_init)
    o_diff = diff_attention(qd[..., 0, :], qd[..., 1, :], kd[..., 0, :], kd[..., 1, :], vd, lam, tbl_diff)
    o_diff = rms_norm(o_diff, diff_out_norm) * (1.0 - lambda_init)
    o_diff = o_diff.reshape(B, S, DIFF_HEADS * DIFF_V_DIM)

    gm = jax.nn.sigmoid(g_merge.reshape(B, S, 2, D_MODEL))
    y = gm[:, :, 0] * (o_nsa @ w_branch_nsa) + gm[:, :, 1] * (o_diff @ w_branch_diff)
    return y @ w_out


def hier_moe(h, rg_w, rg_b, re_w, re_b, w_gate, w_up, w_down):
    B, S, D = h.shape
    hf = h.reshape(B * S, D)
    n_tok = hf.shape[0]
    g_prob = jax.nn.softmax((hf @ rg_w + rg_b).astype(jnp.float32), axis=-1)
    g_top_p, g_top = lax.top_k(g_prob, 1)
    e_logits = (hf @ re_w + re_b).astype(jnp.float32).reshape(n_tok, N_GROUPS, EXPERTS_PER_GROUP)
    e_in = jnp.take_along_axis(e_logits, g_top[:, :, None], axis=1)[:, 0]
    e_prob = jax.nn.softmax(e_in, axis=-1)
    e_top_p, e_top = lax.top_k(e_prob, TOP_K_IN_GROUP)
    e_top_p = e_top_p / jnp.sum(e_top_p, axis=-1, keepdims=True)
    weights = g_top_p * e_top_p
    expert_id = g_top * EXPERTS_PER_GROUP + e_top
    combine = jnp.sum(jax.nn.one_hot(expert_id, N_EXPERTS, dtype=jnp.float32) * weights[..., None], axis=1)
    combine = combine.astype(hf.dtype)
    hid = jax.nn.silu(jnp.einsum('nd,edf->nef', hf, w_gate)) * jnp.einsum('nd,edf->nef', hf, w_up)
    out = jnp.einsum('nef,efd->nd', hid * combine[:, :, None], w_down)
    return out.reshape(B, S, D)


def setup_inputs(seed: int = 0) -> dict:
    key = jax.random.key(seed)
    ks = jax.random.split(key, 24)
    L, dh = DEPTH, HEAD_DIM

    def nrm(k, shape, scale):
        return jax.random.normal(k, shape, jnp.float32) * scale

    def gain(k, shape):
        return 1.0 + 0.1 * jax.random.normal(k, shape, jnp.float32)

    return {
        "x": nrm(ks[0], (BATCH, SEQ, D_MODEL), 1.0),
        "norm1_g": gain(ks[1], (L, D_MODEL)),
        "w_in": nrm(ks[2], (L, D_MODEL, IN_COLS), D_MODEL ** -0.5),
        "nsa_q_norm": gain(ks[3], (L, dh)),
        "nsa_k_norm": gain(ks[4], (L, 3, dh)),
        "cmp_pos": nrm(ks[5], (L, 2, CMP_BLOCK, dh), 0.1),
        "cmp_w1": nrm(ks[6], (L, 2, CMP_BLOCK * dh, CMP_HIDDEN), (CMP_BLOCK * dh) ** -0.5),
        "cmp_w2": nrm(ks[7], (L, 2, CMP_HIDDEN, dh), CMP_HIDDEN ** -0.5),
        "diff_q_norm": gain(ks[8], (L, 2, dh)),
        "diff_k_norm": gain(ks[9], (L, 2, dh)),
        "diff_lambda": nrm(ks[10], (L, 4, dh), 0.1),
        "diff_out_norm": gain(ks[11], (L, DIFF_V_DIM)),
        "rel_bias": nrm(ks[12], (REL_BUCKETS, N_ATTN_HEADS), 0.2),
        "w_branch_nsa": nrm(ks[13], (L, NSA_HEADS * dh, D_MODEL), (NSA_HEADS * dh) ** -0.5),
        "w_branch_diff": nrm(ks[14], (L, DIFF_HEADS * DIFF_V_DIM, D_MODEL), (DIFF_HEADS * DIFF_V_DIM) ** -0.5),
        "w_out": nrm(ks[15], (L, D_MODEL, D_MODEL), D_MODEL ** -0.5),
        "norm2_g": gain(ks[16], (L, D_MODEL)),
        "router_group_w": nrm(ks[17], (L, D_MODEL, N_GROUPS), D_MODEL ** -0.5),
        "router_group_b": nrm(ks[18], (L, N_GROUPS), 0.01),
        "router_expert_w": nrm(ks[19], (L, D_MODEL, N_EXPERTS), D_MODEL ** -0.5),
        "router_expert_b": nrm(ks[20], (L, N_EXPERTS), 0.01),
        "expert_w_gate": nrm(ks[21], (L, N_EXPERTS, D_MODEL, EXPERT_HIDDEN), D_MODEL ** -0.5),
        "expert_w_up": nrm(ks[22], (L, N_EXPERTS, D_MODEL, EXPERT_HIDDEN), D_MODEL ** -0.5),
        "expert_w_down": nrm(ks[23], (L, N_EXPERTS, EXPERT_HIDDEN, D_MODEL), EXPERT_HIDDEN ** -0.5),
    }


def reference(x, norm1_g, w_in, nsa_q_norm, nsa_k_norm, cmp_pos, cmp_w1, cmp_w2,
              diff_q_norm, diff_k_norm, diff_lambda, diff_out_norm, rel_bias,
              w_branch_nsa, w_branch_diff, w_out, norm2_g, router_group_w,
              router_group_b, router_expert_w, router_expert_b, expert_w_gate,
              expert_w_up, expert_w_down):
    for layer in range(DEPTH):
        lambda_init = 0.8 - 0.6 * math.exp(-0.3 * layer)
        h = rms_norm(x, norm1_g[layer])
        x = x + mixer_layer(h, w_in[layer], nsa_q_norm[layer], nsa_k_norm[layer],
                            cmp_pos[layer], cmp_w1[layer], cmp_w2[layer],
                            diff_q_norm[layer], diff_k_norm[layer], diff_lambda[layer],
                            diff_out_norm[layer], rel_bias, w_branch_nsa[layer],
                            w_branch_diff[layer], w_out[layer], lambda_init)
        h = rms_norm(x, norm2_g[layer])
        x = x + hier_moe(h, router_group_w[layer], router_group_b[layer],
                         router_expert_w[layer], router_expert_b[layer],
                         expert_w_gate[layer], expert_w_up[layer], expert_w_down[layer])
    return x
```

```python
import sys
import contextlib
import math
import numpy as np
import ml_dtypes
import concourse.bass as bass
import concourse.mybir as mybir
from concourse.bass_utils import run_bass_kernel_spmd

F32 = mybir.dt.float32
BF16 = mybir.dt.bfloat16
AF = mybir.ActivationFunctionType
ALU = mybir.AluOpType
AX = mybir.AxisListType
NPBF = ml_dtypes.bfloat16

S = 2048
D = 1024
NT = 16
IN_COLS = 4888
NEGM = -30000.0
SCALE = 0.125
EPS = 1e-6
VC = 2063
LAMBDA_INIT = 0.2
DSZ = {F32: 4, BF16: 2}
BUCKET = 2048


def _dsize(dt):
    return DSZ.get(dt, 4)


def _region(ap):
    name = ap.tensor.name
    dims = ap.ap
    off = ap.offset
    dsz = _dsize(ap.dtype)
    space = str(ap.space)
    if space in ("SB", "PSUM"):
        pstep, pcnt = dims[0]
        p0 = ap.base_partition()
        f0 = off - p0 * pstep if pstep else off
        ext = 0
        for st, cnt in dims[1:]:
            ext += abs(st) * (cnt - 1)
            if st < 0:
                f0 += st * (cnt - 1)
        b0 = f0 * dsz
        b1 = (f0 + ext + 1) * dsz
        if space == "PSUM":
            b0 = (b0 // 2048) * 2048
            b1 = ((b1 + 2047) // 2048) * 2048
            return (name, 0, 128, b0, b1)
        return (name, p0, p0 + pcnt, b0, b1)
    lo = off
    hi = off
    for st, cnt in dims:
        if st >= 0:
            hi += st * (cnt - 1)
        else:
            lo += st * (cnt - 1)
    return (name, 0, 1, lo * dsz, (hi + 1) * dsz)


def _overlap(a, b):
    return a[1] < b[2] and b[1] < a[2] and a[3] < b[4] and b[3] < a[4]


def _covers(w, r):
    return w[1] <= r[1] and r[2] <= w[2] and w[3] <= r[3] and r[4] <= w[4]


class Op:
    __slots__ = ("eng", "emit", "reads", "writes", "deps", "is_dma", "dkey",
                 "idx", "need_inc", "src", "iname", "gall")


class Prog:
    def __init__(self, nc):
        self.nc = nc
        self.ops = []
        self.eng_obj = {"pe": nc.tensor, "dve": nc.vector, "act": nc.scalar,
                        "pool": nc.gpsimd, "sp": nc.sync}

    def add(self, eng, emit, reads=(), writes=(), dma=False, dkey=None, gall=False):
        o = Op()
        o.eng = eng
        o.emit = emit
        o.reads = [_region(a) for a in reads]
        o.writes = [_region(a) for a in writes]
        o.is_dma = dma
        o.dkey = dkey
        o.gall = gall
        o.idx = len(self.ops)
        o.need_inc = dma
        o.deps = ()
        f = sys._getframe(1)
        while f is not None and f.f_code.co_name in Prog.__dict__:
            f = f.f_back
        o.src = f.f_lineno if f is not None else -1
        o.iname = None
        self.ops.append(o)
        return o

    def mm(self, out, lhsT, rhs, start=True, stop=True):
        rd = [lhsT, rhs]
        if not start:
            rd.append(out)
        return self.add("pe", lambda e: e.matmul(out, lhsT, rhs, start=start, stop=stop),
                        rd, [out])

    def tr(self, out, in_, ident):
        return self.add("pe", lambda e: e.transpose(out, in_, ident), [in_, ident], [out])

    def dma(self, out, in_, q="sp", key=None, gall=False):
        if key is None:
            key = out.tensor.name
        return self.add(q, lambda e: e.dma_start(out=out, in_=in_), [in_], [out],
                        dma=True, dkey=key, gall=gall)

    def act(self, out, in_, func, bias=None, scale=None, accum_out=None):
        rd = [in_]
        wr = [out]
        kw = {}
        if bias is not None:
            kw["bias"] = bias
            if not isinstance(bias, (int, float)):
                rd.append(bias)
        if scale is not None:
            kw["scale"] = scale
            if not isinstance(scale, (int, float)):
                rd.append(scale)
        if accum_out is not None:
            kw["accum_out"] = accum_out
            wr.append(accum_out)
        return self.add("act", lambda e: e.activation(out, in_, func, **kw), rd, wr)

    def tt(self, eng, out, in0, in1, op):
        return self.add(eng, lambda e: e.tensor_tensor(out, in0, in1, op), [in0, in1], [out])

    def ts(self, eng, out, in0, s1, s2, op0, op1=None, accum_out=None):
        rd = [in0]
        wr = [out]
        for s in (s1, s2):
            if s is not None and not isinstance(s, (int, float)):
                rd.append(s)
        kw = {}
        if op1 is not None:
            kw["op1"] = op1
        if accum_out is not None:
            kw["accum_out"] = accum_out
            wr.append(accum_out)
        return self.add(eng, lambda e: e.tensor_scalar(out, in0, s1, s2, op0, **kw), rd, wr)

    def stt(self, eng, out, in0, scalar, in1, op0, op1):
        rd = [in0, in1]
        if not isinstance(scalar, (int, float)):
            rd.append(scalar)
        return self.add(eng, lambda e: e.scalar_tensor_tensor(out, in0, scalar, in1, op0, op1),
                        rd, [out])

    def copy(self, eng, out, in_):
        if eng == "act":
            return self.add("act", lambda e: e.copy(out, in_), [in_], [out])
        return self.add(eng, lambda e: e.tensor_copy(out, in_), [in_], [out])

    def memset(self, eng, ap, val):
        return self.add(eng, lambda e: e.memset(ap, val), [], [ap])

    def reduce(self, eng, out, in_, op):
        return self.add(eng, lambda e: e.tensor_reduce(out, in_, AX.X, op), [in_], [out])

    def recip(self, out, in_):
        return self.add("dve", lambda e: e.reciprocal(out, in_), [in_], [out])

    def finish(self, semf):
        ops = self.ops
        hist = {}

        def buckets(r):
            return range(r[3] // BUCKET, (r[4] - 1) // BUCKET + 1)

        for o in ops:
            deps = set()
            for r in o.reads:
                is_ps = (r[0] == "ps")
                for b in buckets(r):
                    for rec in hist.get((r[0], b), ()):
                        if _overlap(rec[0], r) and (rec[2] or (is_ps and ops[rec[1]].eng != o.eng)):
                            deps.add(rec[1])
            for w in o.writes:
                for b in buckets(w):
                    for rec in hist.get((w[0], b), ()):
                        if _overlap(rec[0], w):
                            deps.add(rec[1])
            deps.discard(o.idx)
            for w in o.writes:
                for b in buckets(w):
                    lst = hist.setdefault((w[0], b), [])
                    lst[:] = [rec for rec in lst if not _covers(w, rec[0])]
                    lst.append([w, o.idx, True])
            for r in o.reads:
                for b in buckets(r):
                    lst = hist.setdefault((r[0], b), [])
                    found = False
                    if not o.is_dma:
                        for rec in lst:
                            if (not rec[2]) and rec[0] == r:
                                po = ops[rec[1]]
                                if po.eng == o.eng and not po.is_dma:
                                    rec[1] = o.idx
                                    found = True
                                    break
                    if not found:
                        lst.append([r, o.idx, False])
            o.deps = deps
            for d in deps:
                ops[d].need_inc = True
        tl_cnt = {e: 0 for e in ("pe", "dve", "act", "pool")}
        tl_sem = {e: semf("tl_" + e) for e in ("pe", "dve", "act", "pool")}
        dma_sem = {}
        dma_cnt = {}
        token = [None] * len(ops)
        gall_ops = {}
        for o in ops:
            if o.is_dma:
                k = o.dkey
                if k not in dma_sem:
                    dma_sem[k] = semf("d_" + k)
                    dma_cnt[k] = 0
                dma_cnt[k] += 16
                token[o.idx] = [dma_sem[k], dma_cnt[k], "d_" + k]
                if o.gall:
                    gall_ops.setdefault((k, o.gall), []).append(o.idx)
            elif o.need_inc:
                tl_cnt[o.eng] += 1
                token[o.idx] = [tl_sem[o.eng], tl_cnt[o.eng], "tl_" + o.eng]
        for k, lst in gall_ops.items():
            mx = max(token[i][1] for i in lst)
            for i in lst:
                token[i][1] = mx
        seen = {e: {} for e in self.eng_obj}
        n_wait = 0
        LOOK = 6
        pe_ops = [o for o in ops if o.eng == "pe"]
        pe_pos = {o.idx: i for i, o in enumerate(pe_ops)}
        for o in ops:
            e = self.eng_obj[o.eng]
            need = {}
            for d in o.deps:
                p = ops[d]
                if (not p.is_dma) and p.eng == "pe" and o.eng == "pe" and not o.is_dma:
                    continue
                sem, val, key = token[d]
                if need.get(key, (None, 0))[1] < val:
                    need[key] = (sem, val)
            if o.eng == "pe" and need:
                i0_ = pe_pos[o.idx]
                for o2 in pe_ops[i0_ + 1:i0_ + 1 + LOOK]:
                    for d in o2.deps:
                        if d >= o.idx:
                            continue
                        p = ops[d]
                        if (not p.is_dma) and p.eng == "pe":
                            continue
                        sem, val, key = token[d]
                        if key in need and need[key][1] < val:
                            need[key] = (sem, val)
            for key, (sem, val) in need.items():
                if seen[o.eng].get(key, 0) >= val:
                    continue
                seen[o.eng][key] = val
                e.wait_ge(sem, val)
                n_wait += 1
            ins = o.emit(e)
            try:
                o.iname = ins.ins.name
            except Exception:
                pass
            tk = token[o.idx]
            if tk is not None:
                ins.then_inc(tk[0], 16 if o.is_dma else 1)
        self.final = {k: (dma_sem[k], dma_cnt[k]) for k in dma_sem}
        self.n_wait = n_wait
        self.imap = {o.iname: o.src for o in ops if o.iname}
        return self.final


def _t5_bucket_np(dist):
    n = np.maximum(dist, 0)
    nf = np.maximum(n, 1).astype(np.float32)
    large = 16 + (np.log(nf / np.float32(16)) / np.float32(math.log(8.0))
                  * np.float32(16)).astype(np.int32)
    large = np.minimum(large, 31)
    return np.where(n < 16, n, large)


def make_consts():
    c = {}
    eye = np.eye(128, dtype=np.float32)
    c["ident"] = eye.astype(NPBF)
    c["identf"] = eye
    c["jflip"] = eye[::-1].copy().astype(NPBF)
    j = np.arange(4096)
    dist = j - VC
    bk = _t5_bucket_np(dist)
    oh = np.zeros((33, 2, 4096), np.float32)
    for jj in range(4096):
        d = dist[jj]
        if d < 0:
            oh[32, 0, jj] = 1
            oh[32, 1, jj] = 1
        else:
            oh[bk[jj], 0, jj] = 1
            if d < 512:
                oh[bk[jj], 1, jj] = 1
            else:
                oh[32, 1, jj] = 1
    c["oh"] = oh.reshape(33, 8192).astype(NPBF)
    bsel = np.zeros((32, 2048), np.float32)
    for b in range(32):
        bsel[b, 64 * b:64 * b + 64] = 1
    c["bsel"] = bsel.astype(NPBF)
    q = np.arange(2048)
    cur = q // 64
    blk = np.arange(32)[None, :]
    valid = blk <= cur[:, None]
    forced = (blk == 0) | (blk == cur[:, None]) | (blk == cur[:, None] - 1)
    fc = np.where(valid, np.where(forced, 1e4, 0.0), -1e30).astype(np.float32)
    c["fc"] = fc.reshape(16, 128, 32).transpose(1, 0, 2).copy()
    n_cmp = 127
    c_start = np.arange(n_cmp) * 16
    s_start = np.arange(32) * 64
    lo = np.maximum(s_start[:, None], c_start[None, :])
    hi = np.minimum(s_start[:, None] + 64, c_start[None, :] + 32)
    ovl = (np.maximum(hi - lo, 0) / 32).astype(np.float32)
    ovlT = np.zeros((128, 32), np.float32)
    ovlT[:127] = ovl.T
    c["ovlT"] = ovlT.astype(NPBF)
    return c


class _Stop(Exception):
    pass


def build(nc, dbg=None, stop=None):
    dbg = dbg or set()
    P = Prog(nc)

    def ck(name):
        if stop == name:
            raise _Stop()
    dram_in = {}

    def din(name, shape, dt=F32):
        t = nc.dram_tensor(name, list(shape), dt, kind="ExternalInput").ap()
        dram_in[name] = t
        return t

    x = din("x", [S, D])
    g1T = din("g1T", [128, 8])
    w_in = din("w_in", [D, IN_COLS])
    gq_nsa = din("gq_nsa", [64, 1])
    gk_nsa = din("gk_nsa", [64, 3])
    posT = din("posT", [128, 2, 32])
    cmp_w1 = din("cmp_w1", [2, 2048, 256])
    cmp_w2 = din("cmp_w2", [2, 256, 64])
    gqd = din("gqd", [64, 2])
    gkd = din("gkd", [64, 2])
    dlam = din("dlam", [1, 256])
    gdo = din("gdo", [128, 1])
    rel_bias = din("rel_bias", [32, 12])
    w_bn = din("w_bn", [512, D])
    w_bd = din("w_bd", [512, D])
    w_out = din("w_out", [D, D])
    g2T = din("g2T", [128, 8])
    w_r = din("w_r", [D, 20])
    b_r = din("b_r", [1, 20])
    w_g = din("w_g", [16, D, 512])
    w_u = din("w_u", [16, D, 512])
    w_d = din("w_d", [16, 512, D])
    c_ident = din("ident", [128, 128], BF16)
    c_identf = din("identf", [128, 128], F32)
    c_jflip = din("jflip", [128, 128], BF16)
    c_oh = din("oh", [33, 8192], BF16)
    c_bsel = din("bsel", [32, 2048], BF16)
    c_fc = din("fc", [128, 16, 32], F32)
    c_ovlT = din("ovlT", [128, 32], BF16)
    out = nc.dram_tensor("out", [S, D], F32, kind="ExternalOutput").ap()
    vecs = nc.dram_tensor("vecs", [20, 4096], BF16).ap()
    dbg_outs = {}

    def dump(name, ap, dt=None):
        if name not in dbg:
            return
        shp = list(ap.shape)
        t = nc.dram_tensor("dbg_" + name, shp, dt or ap.dtype, kind="ExternalOutput").ap()
        dbg_outs[name] = t
        P.dma(t, ap, q="sp", key="out")

    ARENA_BYTES = 200 * 1024
    arena = nc.alloc_sbuf_tensor("arena", [128, ARENA_BYTES // 2], BF16)

    def A(off, shape, dt=BF16, parts=128):
        n = 1
        for s_ in shape:
            n *= s_
        nb = n * _dsize(dt)
        assert off % 4 == 0 and off + nb <= ARENA_BYTES, (off, shape)
        v = arena[0:parts, off // 2: (off + nb) // 2]
        if dt != BF16:
            v = v.bitcast(dt)
        if len(shape) == 2:
            v = v.rearrange("p (a b) -> p a b", a=shape[0])
        elif len(shape) == 3:
            v = v.rearrange("p (a b c) -> p a b c", a=shape[0], b=shape[1])
        elif len(shape) == 4:
            v = v.rearrange("p (a b c d) -> p a b c d", a=shape[0], b=shape[1], c=shape[2])
        return v

    cst = nc.alloc_sbuf_tensor("cst", [128, 1024], F32)
    cstb = nc.alloc_sbuf_tensor("cstb", [128, 512], BF16)
    ps = nc.alloc_psum_tensor("ps", [128, 4096], F32)

    def bank(b, n=512, parts=128):
        return ps[0:parts, b * 512: b * 512 + n]

    def bankbf(b, parts=128):
        return ps[0:parts, b * 512:(b + 1) * 512].bitcast(BF16)

    g1T_s = cst[:, 0:8]
    g2T_s = cst[:, 8:16]
    gq_s = cst[0:64, 16:17]
    gk_s = cst[0:64, 17:20]
    gqd_s = cst[0:64, 450:452]
    gkd_s = cst[0:64, 452:454]
    gdo_s = cst[:, 22:23]
    chcol = cst[:, 24:36]
    lamw = cst[:, 36:44]
    neglam = cst[:, 44:45]
    br_s = cst[:, 48:68]
    ss1 = cst[:, 72:88]
    rstd1 = cst[:, 88:104]
    ss2 = cst[:, 104:120]
    rstd2 = cst[:, 120:136]
    dl_s = cst[:, 136:392]
    tblx_f = cst[0:33, 392:404]
    pb_s = cst[:, 404:412]
    tiny = cst[:, 416:448]
    zl = cst[0:1, 512:576].bitcast(BF16)
    zr = cst[0:1, 576:736].bitcast(BF16)
    fc_s = nc.alloc_sbuf_tensor("fc_s", [128, 16, 32], F32)
    ident = cstb[:, 0:128]
    jflip = cstb[:, 128:256]
    ovlT_s = cstb[:, 256:288]
    tblx_b = cstb[0:33, 288:300]
    identf = nc.alloc_sbuf_tensor("identf_s", [128, 128], F32)

    O_HT = 0
    O_OT = 32768
    O_W = 65536
    WSLOT = 8 * 544 * 2
    O_N = O_W + 4 * WSLOT
    hT = A(O_HT, [8, 2048])
    oT = A(O_OT, [8, 2048])
    wslot = [A(O_W + i * WSLOT, [8, 544]) for i in range(4)]
    qT = A(O_N, [8, 2048])
    kslc = A(O_N + 32768, [2, 2048])
    kwin = A(O_N + 40960, [2, 2048])
    vnsa = A(O_N + 49152, [16, 2, 2, 65])
    kcv = A(O_N + 57600, [2, 2048])
    Tf = A(O_N + 65792, [12, 640])
    Tw = A(O_N + 81152, [8, 640])
    gates = A(O_N + 91392, [16, 24], F32)
    vcx = A(O_N + 92928, [2, 97])
    kcT = A(O_N + 93316, [2, 128])
    O_NEND = O_N + 93316 + 512
    xt = [A(O_N + i * 4096, [1024], F32) for i in range(2)]
    xn = [A(O_N + 8192 + i * 2048, [1024]) for i in range(2)]
    junk = A(O_N + 12288, [1024])
    o_acc = [A(O_W + i * 8192, [4, 512], F32) for i in range(2)]
    PT = [A(O_W + 16384 + i * 1024, [512]) for i in range(3)]
    Bc8 = A(O_W + 19456, [8, 512])
    ob = A(O_W + 27648, [4, 512])
    imp = A(O_W + 31744, [2, 4, 32], F32)
    O_S = 198656
    sm = A(O_S, [256], F32)
    qn_b = A(O_S + 1024, [512])
    sq_b = A(O_S + 2048, [512])
    sc2 = A(O_S + 3264, [64], F32)
    stage8 = A(O_S + 3520, [8, 96])
    qn_b2 = A(O_S + 5056, [512])
    dq = ["sp"]

    try:
        P.dma(g1T_s, g1T, key="setup0", gall=True)
        P.dma(g2T_s, g2T, key="setup0", gall=True)
        P.dma(gq_s, gq_nsa, key="setup0", gall=True)
        P.dma(gk_s, gk_nsa, key="setup0", gall=True)
        P.dma(gqd_s, gqd, key="setup0", gall=True)
        P.dma(gkd_s, gkd, key="setup0", gall=True)
        P.dma(gdo_s, gdo, key="setup0", gall=True)
        P.dma(chcol, bass.AP(rel_bias.tensor, 31 * 12, [[0, 128], [1, 12]]), key="setup0", gall=True)
        P.dma(br_s, bass.AP(b_r.tensor, 0, [[0, 128], [1, 20]]), key="setup0", gall=True)
        P.dma(dl_s, bass.AP(dlam.tensor, 0, [[0, 128], [1, 256]]), key="setup0", gall=True)
        P.dma(tblx_f[0:32, :], rel_bias, key="setup0", gall=True)
        P.dma(fc_s[:], c_fc, key="setup0", gall=True)
        P.dma(ident, c_ident, key="setup0", gall=True)
        P.dma(jflip, c_jflip, key="setup0", gall=True)
        P.dma(ovlT_s, c_ovlT, key="setup0", gall=True)
        P.dma(identf[:], c_identf, key="setup0", gall=True)
        P.dma(kslc[64:96, 0, :], c_bsel, key="setup0", gall=True)
        P.dma(kslc[64:96, 1, :], c_bsel, key="setup0", gall=True)
        oh_s = A(O_OT, [8192], parts=33)
        P.dma(oh_s, c_oh, key="setup0", gall=True)
        P.memset("dve", tblx_f[32:33, :], NEGM)
        P.memset("dve", tiny, 1e-30)
        P.memset("dve", cst[:, 512:1024], 0.0)
        P.copy("dve", tblx_b, tblx_f)
        P.ts("dve", gq_s, gq_s, SCALE, None, ALU.mult)
        P.ts("dve", gqd_s, gqd_s, SCALE, None, ALU.mult)
        P.ts("dve", gdo_s, gdo_s, 1.0 - LAMBDA_INIT, None, ALU.mult)
        P.tt("dve", sm[:, 0:64], dl_s[:, 0:64], dl_s[:, 64:128], ALU.mult)
        P.reduce("dve", lamw[:, 0:1], sm[:, 0:64], ALU.add)
        P.tt("dve", sm[:, 64:128], dl_s[:, 128:192], dl_s[:, 192:256], ALU.mult)
        P.reduce("dve", lamw[:, 1:2], sm[:, 64:128], ALU.add)
        P.act(lamw[:, 2:4], lamw[:, 0:2], AF.Exp)
        P.tt("dve", lamw[:, 4:5], lamw[:, 3:4], lamw[:, 2:3], ALU.subtract)
        P.ts("dve", neglam, lamw[:, 4:5], -LAMBDA_INIT, None, ALU.add)
        ck("setup")
        w_in_v = w_in.rearrange("(c p) n -> p c n", p=128)
        wctr = [0]

        def load_w(c0, n):
            i = wctr[0] % 4
            wctr[0] += 1
            P.dma(wslot[i][:, :, 0:n], w_in_v[:, :, c0:c0 + n], q="pool", key="w%d" % i)
            return wslot[i]

        wbA = load_w(0, 512)
        wbC = load_w(512, 256)
        def rms_stats(ssum_ap, rstd_ap, n):
            P.act(rstd_ap, ssum_ap, AF.Ln, scale=1.0 / n, bias=eps_col[0:rstd_ap.shape[0], :])
            P.act(rstd_ap, rstd_ap, AF.Exp, scale=-0.5)

        eps_col = cst[:, 448:449]
        P.memset("dve", eps_col, EPS)

        def skew(n, stage1, stage2, depth=1):
            for t_ in range(min(depth, n)):
                stage1(t_)
            for t_ in range(n):
                if t_ + depth < n:
                    stage1(t_ + depth)
                stage2(t_)

        def a_s1(t):
            sl = t % 2
            P.dma(xt[sl], x[128 * t:128 * t + 128, :], key="x%d" % sl)
            P.act(junk, xt[sl], AF.Square, accum_out=ss1[:, t:t + 1])
            rms_stats(ss1[:, t:t + 1], rstd1[:, t:t + 1], D)
            P.ts("dve", xn[sl], xt[sl], rstd1[:, t:t + 1], None, ALU.mult)

        def a_s2(t):
            sl = t % 2
            b0 = 2 + 2 * (t % 2)
            pst = ps[:, b0 * 512:(b0 + 2) * 512].rearrange("p (c k) -> p c k", c=8)
            for c in range(8):
                P.mm(pst[:, c, :], xn[sl][:, 128 * c:128 * c + 128], ident)
            for c in range(8):
                dst = hT[:, c, 128 * t:128 * t + 128]
                if c % 2 == 0:
                    P.ts("dve", dst, pst[:, c, :], g1T_s[:, c:c + 1], None, ALU.mult)
                else:
                    P.act(dst, pst[:, c, :], AF.Copy, scale=g1T_s[:, c:c + 1])
        skew(NT, a_s1, a_s2)
        dump("hT", hT)
        ck("A")
        vsb = A(O_OT + 16384, [4096], parts=12)
        vwb = A(O_OT + 24576, [4096], parts=8)
        for kind, nh, dst in ((0, 12, vsb), (1, 8, vwb)):
            for f in range(8):
                pb = bank(f % 2, 512, parts=nh)
                P.mm(pb, tblx_b[:, 0:nh], oh_s[:, kind * 4096 + f * 512: kind * 4096 + (f + 1) * 512])
                P.copy("dve" if f % 2 else "act", dst[:, f * 512:(f + 1) * 512], pb)
        P.dma(vecs[0:12, :], vsb, q="pool", key="vecs", gall=True)
        P.dma(vecs[12:20, :], vwb, q="pool", key="vecs", gall=True)
        for h in range(12):
            P.dma(Tf[:, h, :], bass.AP(vecs.tensor, h * 4096 + VC - 127, [[1, 128], [1, 640]]),
                  q="pool", key="toep", gall=True)
        for h in range(8):
            P.dma(Tw[:, h, :], bass.AP(vecs.tensor, (12 + h) * 4096 + VC - 127, [[1, 128], [1, 640]]),
                  q="pool", key="toep", gall=True)


        def proj_tok(t, wb, c0, n, pbank):
            for c in range(8):
                P.mm(pbank[:, 0:n], hT[:, c, 128 * t:128 * t + 128], wb[:, c, c0:c0 + n],
                     start=(c == 0), stop=(c == 7))

        def norm_heads(psb, nh, dst3, sel=None):
            P.act(sq_b[:, 0:nh * 64], psb, AF.Square)
            ssum = sm[:, 128:128 + nh]
            rstd = sm[:, 144:144 + nh]
            P.reduce("dve", ssum, sq_b[:, 0:nh * 64].rearrange("p (h d) -> p h d", d=64), ALU.add)
            rms_stats(ssum, rstd, 64)
            src = psb.rearrange("p (h d) -> p h d", d=64)
            r3 = rstd
            if sel is not None:
                src = src[:, sel, :]
                r3 = rstd[:, sel]
            k = src.shape[1]
            P.tt("dve", dst3, src, r3.unsqueeze(2).to_broadcast([128, k, 64]), ALU.mult)

        P.memset("pool", vnsa[:, :, :, :, 64:65], 1.0)

        wb = wbA
        wb_next = wbC
        qnb = [qn_b, qn_b2]

        def ba_s1(t, wb=wb):
            pb = bank(t % 3)
            proj_tok(t, wb, 0, 512, pb)
            norm_heads(pb, 8, qnb[t % 2].rearrange("p (h d) -> p h d", d=64))

        def ba_s2(t):
            b0 = 3 + 2 * (t % 2)
            pst = ps[0:64, b0 * 512:(b0 + 2) * 512].rearrange("p (h k) -> p h k", h=8)
            for h in range(8):
                P.mm(pst[:, h, :], qnb[t % 2][:, 64 * h:64 * h + 64], ident)
            P.ts("dve", qT[0:64, :, 128 * t:128 * t + 128], pst, gq_s, None, ALU.mult)
        skew(NT, ba_s1, ba_s2)
        wb = wb_next
        wb_next = load_w(768, 536)
        for m in range(2):
            for Q in range(4):
                pb = bank((m * 4 + Q) % 2)
                for c in range(8):
                    P.mm(pb, wb[:, c, 128 * m:128 * m + 128], hT[:, c, 512 * Q:512 * Q + 512],
                         start=(c == 0), stop=(c == 7))
                P.copy("act" if Q % 2 else "dve", kcv[:, m, 512 * Q:512 * Q + 512], pb)
        wb = wb_next
        gate_ps = bank(7, 384).rearrange("p (t g) -> p t g", g=24)

        def bb_s1(t, wb=wb):
            pb = bank(t % 3)
            proj_tok(t, wb, 0, 512, pb)
            for c in range(8):
                P.mm(gate_ps[:, t, :], hT[:, c, 128 * t:128 * t + 128], wb[:, c, 512:536],
                     start=(c == 0), stop=(c == 7))
            kn = qnb[t % 2][:, 0:256].rearrange("p (h d) -> p h d", d=64)
            P.act(sq_b, pb, AF.Square)
            ssum = sm[:, 128:136]
            rstd = sm[:, 144:152]
            P.reduce("dve", ssum, sq_b.rearrange("p (h d) -> p h d", d=64), ALU.add)
            rms_stats(ssum, rstd, 64)
            p3 = pb.rearrange("p (h d) -> p h d", d=64)
            for br in range(2):
                P.tt("dve", kn[:, 2 * br:2 * br + 2, :], p3[:, 4 * br:4 * br + 2, :],
                     rstd[:, 4 * br:4 * br + 2].unsqueeze(2).to_broadcast([128, 2, 64]), ALU.mult)
                P.copy("dve", vnsa[:, t, br, :, 0:64], p3[:, 4 * br + 2:4 * br + 4, :])

        def bb_s2(t):
            pst = bank(3 + 2 * (t % 2), parts=64).rearrange("p (h k) -> p h k", h=4)
            for j in range(4):
                P.mm(pst[:, j, :], qnb[t % 2][:, 64 * j:64 * j + 64], ident)
            P.ts("dve", kslc[0:64, :, 128 * t:128 * t + 128], pst[:, 0:2, :], gk_s[:, 1:2], None, ALU.mult)
            P.ts("dve", kwin[0:64, :, 128 * t:128 * t + 128], pst[:, 2:4, :], gk_s[:, 2:3], None, ALU.mult)
        skew(NT, bb_s1, bb_s2)
        gflat = gates.rearrange("p t g -> p (t g)")
        P.act(gflat, bank(7, 384), AF.Exp, scale=-1.0)
        P.ts("dve", gflat, gflat, 1.0, None, ALU.add)
        P.recip(gflat, gflat)
        dump("qT", qT)
        dump("kslc", kslc)
        dump("kwin", kwin)
        dump("vnsa", vnsa)
        dump("gates", gates)

        ck("B")
        w1s = [A(O_W + i * 16384, [32, 256]) for i in range(2)]
        w2s = A(O_OT, [2, 2, 64])
        zs = A(O_OT + 1024, [8, 128], F32)
        z2 = A(O_OT + 5120, [8, 128], F32)
        hidT = A(O_OT + 9216, [8, 128])
        for m in range(2):
            src = cmp_w1[m].rearrange("(l d) c -> d l c", d=64)
            for l0 in range(0, 32, 8):
                P.dma(w1s[m][0:64, l0:l0 + 8, :], src[:, l0:l0 + 8, :], q="pool", key="w1_%d" % m, gall=True)
            P.dma(w2s[:, m, :, :], cmp_w2[m].rearrange("(cc p) d -> p cc d", p=128), q="pool", key="w2s", gall=True)
        posf = A(O_OT + 11776, [64], F32)
        P.dma(posf, posT.rearrange("p m l -> p (m l)"), key="posb")
        tokl = A(O_OT + 12288, [32, 128], parts=64)
        kcv1 = A(O_OT + 20480, [2, 2048], parts=64)
        for m in range(2):
            P.dma(kcv1[:, m, :], kcv[64:128, m, :], key="kcv1", gall=True)
        P.memset("pool", tokl, 0.0)
        ck("c1")
        hps = ps[:, 6 * 512:8 * 512].rearrange("p (i n) -> p i n", n=128)
        for m in range(2):
            for g in range(2):
                srcg = kcv[0:64] if g == 0 else kcv1
                for l in range(32):
                    P.ts("dve", tokl[:, l, 0:127], srcg[:, m, l:l + 16 * 126 + 1:16],
                         posf[0:64, m * 32 + l:m * 32 + l + 1], None, ALU.add)
                for cc in range(2):
                    i = (m * 2 + g) * 2 + cc
                    for l in range(32):
                        P.mm(hps[:, i, :], w1s[m][0:64, l, 128 * cc:128 * cc + 128],
                             tokl[:, l, :], start=(l == 0), stop=(l == 31))
        for i2 in range(2):
            P.copy("act", zs[:, 4 * i2:4 * i2 + 4, :], hps[:, 4 * i2:4 * i2 + 4, :])
        ck("c2")
        zf = zs.rearrange("p i n -> p (i n)")
        z2f = z2.rearrange("p i n -> p (i n)")
        P.tt("dve", z2f, zf, zf, ALU.mult)
        P.ts("dve", z2f, z2f, 0.044715, 1.0, ALU.mult, ALU.add)
        P.tt("dve", z2f, z2f, zf, ALU.mult)
        P.act(z2f, z2f, AF.Exp, scale=-2.0 * math.sqrt(2.0 / math.pi))
        P.ts("dve", z2f, z2f, 1.0, None, ALU.add)
        P.recip(z2f, z2f)
        P.tt("dve", hidT.rearrange("p i n -> p (i n)"), z2f, zf, ALU.mult)
        ck("c3")
        cps = bank(5, 256).rearrange("p (i d) -> p i d", d=64)
        for m in range(2):
            for g in range(2):
                for cc in range(2):
                    i = (m * 2 + g) * 2 + cc
                    P.mm(cps[:, m * 2 + g, :], hidT[:, i, :], w2s[:, m, cc, :],
                         start=(cc == 0), stop=(cc == 1))
        P.act(sq_b[:, 0:128], cps[:, 0:2, :].rearrange("p i d -> p (i d)"), AF.Square)
        P.reduce("dve", sm[:, 128:130], sq_b[:, 0:128].rearrange("p (h d) -> p h d", d=64), ALU.add)
        rms_stats(sm[:, 128:130], sm[:, 144:146], 64)
        kcn = qn_b[:, 0:128].rearrange("p (h d) -> p h d", d=64)
        P.tt("dve", kcn, cps[:, 0:2, :], sm[:, 144:146].unsqueeze(2).to_broadcast([128, 2, 64]), ALU.mult)
        P.copy("act", vcx[:, :, 0:64], cps[:, 2:4, :])
        P.memset("dve", vcx[:, :, 64:65], 1.0)
        for g in range(2):
            P.copy("dve", vcx[:, g, 65:97], ovlT_s)
        pst = bank(3, parts=64).rearrange("p (h k) -> p h k", h=4)
        for g in range(2):
            P.mm(pst[:, g, :], qn_b[:, 64 * g:64 * g + 64], ident)
        P.ts("dve", kcT[0:64, :, :], pst[:, 0:2, :], gk_s[:, 0:1], None, ALU.mult)
        dump("kcT", kcT)
        dump("vcx", vcx)

        ck("cmp")
        P.memset("dve", stage8[:, :, 0:64], 0.0)

        def run_items(items, depth=2):
            n = len(items)
            deferred = []

            def defer(k, delay, fn):
                deferred.append([k + delay, fn])

            def flush(k):
                rest = []
                for d_ in list(deferred):
                    if d_[0] <= k:
                        d_[1]()
                    else:
                        rest.append(d_)
                deferred[:] = rest

            def issueS(k):
                it = items[k]
                if it.get("pre"):
                    it["pre"]()
                it["S"](k)
            for k in range(min(depth, n)):
                issueS(k)
            for k in range(n):
                flush(k)
                items[k]["E"](k)
                if k + depth < n:
                    issueS(k + depth)
                items[k]["V"](k)
                if items[k].get("post"):
                    items[k]["post"](k, defer)
            flush(10 ** 9)

        def attn_items(items, Q, kT_ap, q_ap, Ttab, th, vfun, vw, accf, jlo_fun, use_T_always,
                       pre_first=None, post_last=None, pts=None):
            pts = pts or PT
            i0 = 4 * Q
            started = set()
            last_of_bank = {}
            for tt_ in range(4):
                last_of_bank[_region(accf(tt_))[3] // 2048] = tt_
            jmin = min(jlo_fun(i) for i in range(i0, i0 + 4))
            first = len(items)
            for j in range(jmin, i0 + 4):
                tiles = [i for i in range(i0, i0 + 4) if jlo_fun(i) <= j <= i]
                i_lo, i_hi = tiles[0], tiles[-1]
                n = (i_hi - i_lo + 1) * 128
                c0 = (i_lo - i0) * 128
                need_T = use_T_always or (j >= i0 - 1)
                x0 = (i_lo - j) * 128

                def fS(k, j=j, n=n, c0=c0, need_T=need_T, x0=x0):
                    sb = bank(k % 3)
                    P.mm(sb[:, 0:n], kT_ap(j), q_ap(c0, n), start=True, stop=not need_T)
                    if need_T:
                        P.mm(sb[:, 0:n], jflip, Ttab[:, th, x0:x0 + n], start=False, stop=True)

                def fE(k, n=n, need_T=need_T):
                    sb = bank(k % 3)
                    pt = pts[k % 3]
                    if need_T:
                        P.act(pt[:, 0:n], sb[:, 0:n], AF.Exp)
                    else:
                        P.act(pt[:, 0:n], sb[:, 0:n], AF.Exp, bias=chcol[:, th:th + 1])

                def fV(k, j=j, tiles=tiles, i_lo=i_lo):
                    pt = pts[k % 3]
                    for i in tiles:
                        oap = accf(i - i0)
                        bk_ = _region(oap)[3] // 2048
                        P.mm(oap, pt[:, (i - i_lo) * 128:(i - i_lo) * 128 + 128], vfun(j),
                             start=(bk_ not in started), stop=(j == i and i - i0 == last_of_bank[bk_]))
                        started.add(bk_)
                items.append(dict(S=fS, E=fE, V=fV))
            items[first]["pre"] = pre_first
            items[-1]["post"] = post_last

        def finalize_nsa(acc, Q, h, br):
            i0 = 4 * Q
            oa = o_acc[Q % 2]
            rs = sm[:, 160:164]
            P.ts("dve", rs, acc[:, :, 64], 1e-30, None, ALU.max)
            P.recip(rs, rs)
            P.tt("dve", rs, rs, gates[:, i0:i0 + 4, br * 8 + h], ALU.mult)
            for tt_ in range(4):
                dst = oa[:, tt_, 64 * h:64 * h + 64]
                P.stt("dve", dst, acc[:, tt_, 0:64], rs[:, tt_:tt_ + 1], dst, ALU.mult, ALU.add)

        acc_rot = [5, 6, 4, 7]
        accc = [0]

        def next_acc():
            b_ = acc_rot[accc[0] % 4]
            accc[0] += 1
            return b_

        def load_bc(Q):
            for h in range(8):
                P.dma(Bc8[:, h, :], bass.AP(vecs.tensor, h * 4096 + 512 * Q, [[16, 128], [1, 512]]),
                      key="bc8", gall="q%d" % Q)

        def sel_dve(Q):
            i0 = 4 * Q
            for g in range(2):
                for tt_ in range(4):
                    t = i0 + tt_
                    sc = sc2[:, 0:32]
                    m8 = sc2[:, 32:48]
                    scr = sm[:, 192:224]
                    P.tt("dve", sc, imp[:, g, tt_, :], fc_s[:, t, :], ALU.add)
                    P.add("dve", lambda e, m8=m8, sc=sc: e.max(out=m8[:, 0:8], in_=sc), [sc], [m8[:, 0:8]])
                    P.add("dve", lambda e, m8=m8, sc=sc, scr=scr: e.match_replace(
                        out=scr, in_to_replace=m8[:, 0:8], in_values=sc, imm_value=-3e38),
                        [m8[:, 0:8], sc], [scr])
                    P.add("dve", lambda e, m8=m8, scr=scr: e.max(out=m8[:, 8:16], in_=scr), [scr], [m8[:, 8:16]])
                    P.ts("dve", stage8[:, g * 4 + tt_, 64:96], sc, m8[:, 15:16], NEGM, ALU.is_lt, ALU.mult)

        def sel_pe(Q):
            i0 = 4 * Q
            for g in range(2):
                pst = bank(3, 512, parts=96)
                for tt_ in range(4):
                    P.mm(pst[:, 128 * tt_:128 * tt_ + 128], stage8[:, g * 4 + tt_, 0:96], ident)
                P.copy("act", qT[64:96, 4 * g:4 * g + 4, 512 * Q:512 * Q + 512],
                       pst[64:96, :].unsqueeze(1).to_broadcast([32, 4, 512]))

        def conv_a(Q):
            for tt_ in range(4):
                P.copy("dve", ob[:, tt_, :], o_acc[Q % 2][:, tt_, :])

        def conv_b(Q):
            for tt_ in range(4):
                t = 4 * Q + tt_
                pst = bank(3).rearrange("p (c k) -> p c k", c=4)
                for k_ in range(4):
                    P.mm(pst[:, k_, :], ob[:, tt_, 128 * k_:128 * k_ + 128], ident)
                P.copy("dve", oT[:, 0:4, 128 * t:128 * t + 128], pst[:, 0:4, :])

        items = []
        load_bc(0)
        for Q in range(4):
            i0 = 4 * Q
            qs = slice(512 * Q, 512 * Q + 512)
            for h in range(8):
                g = h // 4
                acc = bank(next_acc(), 4 * 97).rearrange("p (t v) -> p t v", v=97)

                def fS(k, g=g, h=h, qs=qs):
                    sb = bank(k % 3)
                    P.mm(sb, kcT[0:64, g, :], qT[0:64, h, qs], start=True, stop=False)
                    P.mm(sb, jflip, Bc8[:, h, :], start=False, stop=True)

                def fE(k):
                    P.act(PT[k % 3], bank(k % 3), AF.Exp)

                def fV(k, acc=acc, g=g):
                    for tt_ in range(4):
                        P.mm(acc[:, tt_, :], PT[k % 3][:, 128 * tt_:128 * tt_ + 128], vcx[:, g, :])

                def post(k, defer, acc=acc, g=g, h=h, Q=Q, i0=i0):
                    rs = sm[:, 160:164]
                    P.ts("dve", rs, acc[:, :, 64], 1e-30, None, ALU.max)
                    P.recip(rs, rs)
                    for tt_ in range(4):
                        dsti = imp[:, g, tt_, :]
                        if h % 4 == 0:
                            P.ts("dve", dsti, acc[:, tt_, 65:97], rs[:, tt_:tt_ + 1], None, ALU.mult)
                        else:
                            P.stt("dve", dsti, acc[:, tt_, 65:97], rs[:, tt_:tt_ + 1], dsti, ALU.mult, ALU.add)
                    P.tt("dve", rs, rs, gates[:, i0:i0 + 4, h], ALU.mult)
                    for tt_ in range(4):
                        P.ts("dve", o_acc[Q % 2][:, tt_, 64 * h:64 * h + 64], acc[:, tt_, 0:64],
                             rs[:, tt_:tt_ + 1], None, ALU.mult)
                    if h == 7:
                        sel_dve(Q)
                        if Q + 1 < 4:
                            load_bc(Q + 1)
                items.append(dict(S=fS, E=fE, V=fV, post=post))
            for h in range(8):
                g = h // 4
                acc = bank(next_acc(), 4 * 65).rearrange("p (t v) -> p t v", v=65)
                attn_items(items, Q,
                           lambda j, g=g: kwin[0:64, g, 128 * j:128 * j + 128],
                           lambda c0, n, h=h, Q=Q: qT[0:64, h, 512 * Q + c0:512 * Q + c0 + n],
                           Tw, h, lambda j, g=g: vnsa[:, j, 1, g, :], 65,
                           lambda tt_, acc=acc: acc[:, tt_, 0:65],
                           lambda i: max(0, i - 4), True,
                           post_last=lambda k, defer, acc=acc, Q=Q, h=h: finalize_nsa(acc, Q, h, 2))
            for h in range(8):
                g = h // 4
                acc = bank(next_acc(), 4 * 65).rearrange("p (t v) -> p t v", v=65)
                if h == 7:
                    def post_l(k, defer, acc=acc, Q=Q, h=h):
                        finalize_nsa(acc, Q, h, 1)
                        defer(k, 2, lambda Q=Q: conv_a(Q))
                        defer(k, 6, lambda Q=Q: conv_b(Q))
                else:
                    def post_l(k, defer, acc=acc, Q=Q, h=h):
                        finalize_nsa(acc, Q, h, 1)
                attn_items(items, Q,
                           lambda j, g=g: kslc[0:96, g, 128 * j:128 * j + 128],
                           lambda c0, n, h=h, Q=Q: qT[0:96, h, 512 * Q + c0:512 * Q + c0 + n],
                           Tf, h, lambda j, g=g: vnsa[:, j, 0, g, :], 65,
                           lambda tt_, acc=acc: acc[:, tt_, 0:65],
                           lambda i: 0, False,
                           pre_first=(lambda Q=Q: sel_pe(Q)) if h == 0 else None,
                           post_last=post_l)
        run_items(items)
        dump("qTm", qT)
        dump("oTn", oT)

        ck("nsa")
        qd = A(O_N, [8, 2048])
        kd = A(O_N + 32768, [8, 2048])
        vd = A(O_N + 81152, [16, 4, 129])
        P.memset("pool", vd[:, :, :, 128:129], 1.0)
        wb = load_w(1304, 512)
        wb_next = load_w(1816, 512)
        for blk in range(3):
            if blk < 2:
                dstT = qd if blk == 0 else kd
                gsc = gqd_s if blk == 0 else gkd_s

                def d_s1(t, wb=wb):
                    pb = bank(t % 3)
                    proj_tok(t, wb, 0, 512, pb)
                    norm_heads(pb, 8, qnb[t % 2].rearrange("p (h d) -> p h d", d=64))

                def d_s2(t, dstT=dstT, gsc=gsc):
                    b0 = 3 + 2 * (t % 2)
                    pst = ps[0:64, b0 * 512:(b0 + 2) * 512].rearrange("p (h k) -> p h k", h=8)
                    for hh in range(8):
                        P.mm(pst[:, hh, :], qnb[t % 2][:, 64 * hh:64 * hh + 64], ident)
                    for i_ in range(2):
                        P.ts("dve", dstT[0:64, i_::2, 128 * t:128 * t + 128], pst[:, i_::2, :],
                             gsc[:, i_:i_ + 1], None, ALU.mult)
                skew(NT, d_s1, d_s2)
            else:
                for t in range(NT):
                    pb = bank(t % 3)
                    proj_tok(t, wb, 0, 512, pb)
                    P.copy("act" if t % 2 else "dve", vd[:, t, :, 0:128], pb.rearrange("p (h d) -> p h d", d=128))
            if blk == 0:
                wb = wb_next
                wb_next = load_w(2328, 512)
            elif blk == 1:
                wb = wb_next
        dump("qd", qd)
        dump("kd", kd)

        O_DT = O_N + 65792
        PTd = [A(O_DT + i * 1024, [512]) for i in range(3)]
        o1s = [A(O_DT + 3072 + i * 2048, [4, 128], F32) for i in range(2)]
        obd = [A(O_DT + 7168 + i * 1024, [4, 128]) for i in range(2)]
        GM0 = 2840
        blocks = {}
        P.dma(wslot[0][:, :, 0:512], w_in_v[:, :, GM0:GM0 + 512], q="pool", key="w0")
        P.dma(wslot[1][:, :, 0:512], w_in_v[:, :, GM0 + 1024:GM0 + 1536], q="pool", key="w1")
        blocks[(0, 0)] = wslot[0]
        blocks[(1, 0)] = wslot[1]
        wbn = A(O_W + 17408, [4, 1024])
        wbd = A(O_W + 25600, [4, 1024])
        P.dma(wbn, w_bn.rearrange("(k p) n -> p k n", p=128), q="pool", key="wbn")
        P.dma(wbd, w_bd.rearrange("(k p) n -> p k n", p=128), q="pool", key="wbd")
        items = []
        acc_sets = [(5, 6), (4, 7)]
        dcnt = [0]
        for Q in range(4):
            for h in range(4):
                hb = (Q * 4 + h) % 2
                for i_ in range(2):
                    aset = acc_sets[dcnt[0] % 2]
                    dcnt[0] += 1
                    accv = [ps[:, aset[tt_ // 2] * 512 + (tt_ % 2) * 160:aset[tt_ // 2] * 512 + (tt_ % 2) * 160 + 129]
                            for tt_ in range(4)]

                    def stage_a(Q=Q, h=h, hb=hb):
                        for tt_ in range(4):
                            P.act(sq_b[:, 0:128], o1s[hb][:, tt_, :], AF.Square,
                                  accum_out=sm[:, 168 + tt_:169 + tt_])
                        rms_stats(sm[:, 168:172], sm[:, 172:176], 128)
                        for tt_ in range(4):
                            P.ts("dve", obd[hb][:, tt_, :], o1s[hb][:, tt_, :], sm[:, 172 + tt_:173 + tt_],
                                 None, ALU.mult)

                    def stage_b(Q=Q, h=h, hb=hb):
                        pst = bank(3).rearrange("p (c k) -> p c k", c=4)
                        for tt_ in range(4):
                            P.mm(pst[:, tt_, :], obd[hb][:, tt_, :], ident)
                        P.act(oT[:, 4 + h, 512 * Q:512 * Q + 512], pst[:, 0:4, :].rearrange("p c k -> p (c k)"),
                              AF.Copy, scale=gdo_s)

                    def post(k, defer, accv=accv, i_=i_, hb=hb, stage_a=stage_a, stage_b=stage_b):
                        rs = sm[:, 160:164]
                        for tt_ in range(4):
                            P.ts("dve", rs[:, tt_:tt_ + 1], accv[tt_][:, 128:129], 1e-30, None, ALU.max)
                        P.recip(rs, rs)
                        if i_ == 0:
                            for tt_ in range(4):
                                P.ts("dve", o1s[hb][:, tt_, :], accv[tt_][:, 0:128], rs[:, tt_:tt_ + 1],
                                     None, ALU.mult)
                        else:
                            P.ts("dve", rs, rs, neglam[:, 0:1], None, ALU.mult)
                            for tt_ in range(4):
                                P.stt("dve", o1s[hb][:, tt_, :], accv[tt_][:, 0:128], rs[:, tt_:tt_ + 1],
                                      o1s[hb][:, tt_, :], ALU.mult, ALU.add)
                            defer(k, 5, stage_a)
                            defer(k, 10, stage_b)
                    attn_items(items, Q,
                               lambda j, h=h, i_=i_: kd[0:64, 2 * h + i_, 128 * j:128 * j + 128],
                               lambda c0, n, h=h, i_=i_, Q=Q: qd[0:64, 2 * h + i_, 512 * Q + c0:512 * Q + c0 + n],
                               Tf, 8 + h, lambda j, h=h: vd[:, j, h, :], 129,
                               lambda tt_, accv=accv: accv[tt_],
                               lambda i: 0, False, post_last=post, pts=PTd)
        run_items(items)
        dump("oT", oT)

        ck("diff")
        yT = A(O_N, [8, 2048])
        gsc_ = [A(O_N + 49152 + 2048 + i * 2048, [512], F32) for i in range(4)]
        ysc = [A(O_N + 59392 + i * 2048, [512], F32) for i in range(2)]
        gmB = [A(O_N + 32768 + i * WSLOT, [8, 544]) for i in range(2)]
        P.dma(gmB[0][:, :, 0:512], w_in_v[:, :, GM0 + 512:GM0 + 1024], q="pool", key="gmb0")
        P.dma(gmB[1][:, :, 0:512], w_in_v[:, :, GM0 + 1536:GM0 + 2048], q="pool", key="gmb1")
        blocks[(0, 1)] = gmB[0]
        blocks[(1, 1)] = gmB[1]
        wo_v = w_out.rearrange("(c p) n -> p c n", p=128)
        it = 0
        for cc in range(8):
            hb = cc // 4
            co = 128 * (cc % 4)
            if cc == 4:
                for hf in range(2):
                    P.dma(wslot[hf][:, :, 0:512], wo_v[:, :, 512 * hf:512 * hf + 512], q="pool", key="w%d" % hf)
            for Q in range(4):
                qs = slice(512 * Q, 512 * Q + 512)
                pg = [bank(0 + 2 * (it % 2)), bank(1 + 2 * (it % 2))]
                pbm = [bank(4 + 2 * (it % 2)), bank(5 + 2 * (it % 2))]
                for s_ in range(2):
                    wbk = blocks[(s_, hb)]
                    for c in range(8):
                        P.mm(pg[s_], wbk[:, c, co:co + 128], hT[:, c, qs], start=(c == 0), stop=(c == 7))
                for k in range(4):
                    P.mm(pbm[0], wbn[:, k, 128 * cc:128 * cc + 128], oT[:, k, qs], start=(k == 0), stop=(k == 3))
                for k in range(4):
                    P.mm(pbm[1], wbd[:, k, 128 * cc:128 * cc + 128], oT[:, 4 + k, qs], start=(k == 0), stop=(k == 3))
                e0 = gsc_[2 * (it % 2)]
                e1 = gsc_[2 * (it % 2) + 1]
                P.act(e0, pg[0], AF.Sigmoid)
                P.act(e1, pg[1], AF.Sigmoid)
                y0 = ysc[it % 2]
                P.tt("dve", y0, pbm[0], e0, ALU.mult)
                P.tt("dve", e1, pbm[1], e1, ALU.mult)
                P.tt("dve", yT[:, cc, qs], y0, e1, ALU.add)
                it += 1
        dump("yT", yT)

        ck("g1")
        x1 = A(O_N + 32768, [16, 1024], F32)
        h2T = A(O_HT, [8, 2048])
        wo = [wslot[0], wslot[1]]
        xt2 = [A(O_OT + i * 4096, [1024], F32) for i in range(2)]
        xhi = [A(O_OT + 8192 + i * 4096, [1024]) for i in range(2)]
        xlo = [A(O_OT + 8192 + i * 4096 + 2048, [1024]) for i in range(2)]
        hiT = [A(O_OT + 16384 + i * 4096, [8, 128]) for i in range(2)]
        loT = [A(O_OT + 16384 + i * 4096 + 2048, [8, 128]) for i in range(2)]
        whi = A(O_OT + 25216, [8, 20])
        wlo = A(O_OT + 28672, [8, 20])
        wr_s = A(O_OT + 24576, [8, 20], F32)
        junk2 = A(O_OT + 25600, [1024])
        comb = A(O_OT + 27648, [16, 16], F32)
        rsm = A(O_OT + 28672, [128], F32)
        P.dma(wr_s, w_r.rearrange("(c p) n -> p c n", p=128), key="wr")
        P.tt("dve", wr_s, wr_s, g2T_s[:, 0:8].unsqueeze(2).to_broadcast([128, 8, 20]), ALU.mult)
        P.copy("dve", whi, wr_s)
        P.tt("dve", wlo, wr_s, whi, ALU.subtract)
        lgall = A(O_OT + 29184, [16, 20], F32)
        mw = [(A(O_W + 17408, [8, 512]), A(O_W + 25600, [8, 512]), A(O_W, [4, 1024])),
              (A(O_N, [8, 512]), A(O_N + 8192, [8, 512]), A(O_N + 16384, [4, 1024]))]
        hid = [A(O_N + 24576 + i * 4096, [4, 512]) for i in range(2)]
        sg = [A(O_W + 8192 + i * 1024, [512]) for i in range(2)]
        it = 0

        def load_expert(e, parts=(0, 1, 2)):
            wg_, wu_, wd_ = mw[e % 2]
            k = "moe%d" % (e % 2)
            if 0 in parts:
                P.dma(wg_, w_g[e].rearrange("(c p) n -> p c n", p=128), q="pool", key=k, gall="e%d" % e)
            if 1 in parts:
                P.dma(wu_, w_u[e].rearrange("(c p) n -> p c n", p=128), q="pool", key=k, gall="e%d" % e)
            if 2 in parts:
                P.dma(wd_, w_d[e].rearrange("(c p) n -> p c n", p=128), q="pool", key="moed%d" % (e % 2))
        load_expert(0, parts=(0, 1))

        def g2_s1(t):
            sl = t % 2
            P.dma(xt2[sl], x[128 * t:128 * t + 128, :], key="x%d" % sl)
            for hf in range(2):
                pb = bank(hf)
                for cc in range(8):
                    P.mm(pb, yT[:, cc, 128 * t:128 * t + 128], wo[hf][:, cc, 0:512],
                         start=(cc == 0), stop=(cc == 7))
                P.tt("dve", x1[:, t, 512 * hf:512 * hf + 512], pb, xt2[sl][:, 512 * hf:512 * hf + 512], ALU.add)
            P.act(junk2, x1[:, t, :], AF.Square, accum_out=ss2[:, t:t + 1])
            rms_stats(ss2[:, t:t + 1], rstd2[:, t:t + 1], D)
            P.act(xhi[sl], x1[:, t, :], AF.Copy, scale=rstd2[:, t:t + 1])
            P.stt("dve", xlo[sl], x1[:, t, :], rstd2[:, t:t + 1], xhi[sl], ALU.mult, ALU.subtract)

        def g2_s2(t):
            sl = t % 2
            for src, dstT, b0 in ((xhi[sl], hiT[sl], 2), (xlo[sl], loT[sl], 4)):
                for half in range(2):
                    pstf = bank(b0 + half).rearrange("p (c k) -> p c k", c=4)
                    for c4 in range(4):
                        c = half * 4 + c4
                        P.mm(pstf[:, c4, :], src[:, 128 * c:128 * c + 128], ident)
                    P.copy("act" if half else "dve", dstT[:, 4 * half:4 * half + 4, :], pstf)
            P.tt("dve", h2T[:, :, 128 * t:128 * t + 128], hiT[sl],
                 g2T_s[:, 0:8].unsqueeze(2).to_broadcast([128, 8, 128]), ALU.mult)
            pr = ps[:, 6 * 512 + 32 * (t % 2):6 * 512 + 32 * (t % 2) + 20]
            k_ = 0
            for aT, w_ in ((hiT[sl], whi), (hiT[sl], wlo), (loT[sl], whi)):
                for c in range(8):
                    P.mm(pr, aT[:, c, :], w_[:, c, :], start=(k_ == 0), stop=(k_ == 23))
                    k_ += 1
            P.tt("dve", lgall[:, t, :], pr, br_s, ALU.add)
        skew(NT, g2_s1, g2_s2)
        R = A(O_OT + 30464, [576], F32)
        gl = lgall[:, :, 0:4]
        el = lgall[:, :, 4:20].rearrange("p t (g e) -> p t g e", e=4)
        gmax = R[:, 0:16]
        P.reduce("dve", gmax, gl, ALU.max)
        gsh = R[:, 16:80].rearrange("p (t g) -> p t g", g=4)
        P.tt("dve", gsh, gl, gmax.unsqueeze(2).to_broadcast([128, 16, 4]), ALU.subtract)
        goh = R[:, 80:144].rearrange("p (t g) -> p t g", g=4)
        P.tt("dve", goh, gl, gmax.unsqueeze(2).to_broadcast([128, 16, 4]), ALU.is_ge)
        P.act(R[:, 16:80], R[:, 16:80], AF.Exp)
        gsum = R[:, 144:160]
        P.reduce("dve", gsum, gsh, ALU.add)
        P.recip(gsum, gsum)
        msk = R[:, 160:416].rearrange("p (t g e) -> p t g e", g=4, e=4)
        P.tt("dve", msk, el, goh.unsqueeze(3).to_broadcast([128, 16, 4, 4]), ALU.mult)
        ein = R[:, 416:480].rearrange("p (t e) -> p t e", e=4)
        P.reduce("dve", ein, msk.rearrange("p t g e -> p t e g"), ALU.add)
        m1 = R[:, 480:496]
        P.reduce("dve", m1, ein, ALU.max)
        eq1 = R[:, 496:560].rearrange("p (t e) -> p t e", e=4)
        P.tt("dve", eq1, ein, m1.unsqueeze(2).to_broadcast([128, 16, 4]), ALU.is_ge)
        ein2 = R[:, 160:224].rearrange("p (t e) -> p t e", e=4)
        P.stt("dve", ein2, eq1, -1e30, ein, ALU.mult, ALU.add)
        m2 = R[:, 560:576]
        P.reduce("dve", m2, ein2, ALU.max)
        mask2 = R[:, 224:288].rearrange("p (t e) -> p t e", e=4)
        P.tt("dve", mask2, ein, m2.unsqueeze(2).to_broadcast([128, 16, 4]), ALU.is_ge)
        esh = R[:, 288:352].rearrange("p (t e) -> p t e", e=4)
        P.tt("dve", esh, ein, m1.unsqueeze(2).to_broadcast([128, 16, 4]), ALU.subtract)
        P.act(R[:, 288:352], R[:, 288:352], AF.Exp)
        P.tt("dve", esh, esh, mask2, ALU.mult)
        den = R[:, 352:368]
        P.reduce("dve", den, esh, ALU.add)
        P.recip(den, den)
        P.tt("dve", den, den, gsum, ALU.mult)
        P.tt("dve", esh, esh, den.unsqueeze(2).to_broadcast([128, 16, 4]), ALU.mult)
        P.tt("dve", comb.rearrange("p t (g e) -> p t g e", e=4),
             goh.unsqueeze(3).to_broadcast([128, 16, 4, 4]),
             esh.unsqueeze(2).to_broadcast([128, 16, 4, 4]), ALU.mult)
        dump("x1", x1)
        dump("comb", comb)
        dump("h2T", h2T)
        ck("g2")
        load_expert(0, parts=(2,))

        for e in range(16):
            if e + 1 < 16:
                load_expert(e + 1)
            elif False:
                pass
            wg_, wu_, wd_ = mw[e % 2]
            for Q in range(4):
                qs = slice(512 * Q, 512 * Q + 512)
                hb = hid[it % 2]
                for fc in range(4):
                    pgb = bank(0 + (fc % 2) * 2)
                    pub = bank(1 + (fc % 2) * 2)
                    for c in range(8):
                        P.mm(pgb, wg_[:, c, 128 * fc:128 * fc + 128], h2T[:, c, qs], start=(c == 0), stop=(c == 7))
                    for c in range(8):
                        P.mm(pub, wu_[:, c, 128 * fc:128 * fc + 128], h2T[:, c, qs], start=(c == 0), stop=(c == 7))
                    sgb = sg[fc % 2]
                    P.act(sgb, pgb, AF.Silu)
                    P.tt("dve", hb[:, fc, :], sgb, pub, ALU.mult)
                for tt_ in range(4):
                    t = 4 * Q + tt_
                    for hf in range(2):
                        po = bank(4 + (2 * tt_ + hf) % 4)
                        for fc in range(4):
                            P.mm(po, hb[:, fc, 128 * tt_:128 * tt_ + 128], wd_[:, fc, 512 * hf:512 * hf + 512],
                                 start=(fc == 0), stop=(fc == 3))
                        dst = x1[:, t, 512 * hf:512 * hf + 512]
                        P.stt("dve", dst, po, comb[:, t, e:e + 1], dst, ALU.mult, ALU.add)
                    if e == 15:
                        P.dma(out[128 * t:128 * t + 128, :], x1[:, t, :], key="out")
                it += 1

    except _Stop:
        pass
    es = contextlib.ExitStack()

    def semf(name):
        return es.enter_context(nc.semaphore(name))
    fin = P.finish(semf)
    for k, (s_, v) in fin.items():
        nc.sync.wait_ge(s_, v)
    es.close()
    return P, dram_in, dbg_outs


def prep_shared(inputs):
    f = lambda a: np.ascontiguousarray(np.asarray(a, dtype=np.float32))
    d = {}
    d["g1T"] = f(inputs["norm1_g"][0].reshape(8, 128).T)
    d["w_in"] = f(inputs["w_in"][0])
    d["gq_nsa"] = f(inputs["nsa_q_norm"][0].reshape(64, 1))
    d["gk_nsa"] = f(inputs["nsa_k_norm"][0].T)
    pos = np.asarray(inputs["cmp_pos"][0], np.float32)
    pT = np.transpose(pos, (2, 0, 1))
    d["posT"] = f(np.concatenate([pT, pT], axis=0))
    d["cmp_w1"] = f(inputs["cmp_w1"][0])
    d["cmp_w2"] = f(inputs["cmp_w2"][0])
    d["gqd"] = f(inputs["diff_q_norm"][0].T)
    d["gkd"] = f(inputs["diff_k_norm"][0].T)
    d["dlam"] = f(inputs["diff_lambda"][0].reshape(1, 256))
    d["gdo"] = f(inputs["diff_out_norm"][0].reshape(128, 1))
    d["rel_bias"] = f(inputs["rel_bias"])
    d["w_bn"] = f(inputs["w_branch_nsa"][0])
    d["w_bd"] = f(inputs["w_branch_diff"][0])
    d["w_out"] = f(inputs["w_out"][0])
    d["g2T"] = f(inputs["norm2_g"][0].reshape(8, 128).T)
    d["w_r"] = f(np.concatenate([inputs["router_group_w"][0], inputs["router_expert_w"][0]], axis=1))
    d["b_r"] = f(np.concatenate([inputs["router_group_b"][0], inputs["router_expert_b"][0]]).reshape(1, 20))
    d["w_g"] = f(inputs["expert_w_gate"][0])
    d["w_u"] = f(inputs["expert_w_up"][0])
    d["w_d"] = f(inputs["expert_w_down"][0])
    d.update(make_consts())
    return d


def kernel(**inputs):
    nc = bass.Bass("TRN2", target_bir_lowering=False)
    build(nc)
    shared = prep_shared(inputs)
    xs = np.asarray(inputs["x"], np.float32)
    in_maps = []
    for b in range(8):
        m = dict(shared)
        m["x"] = np.ascontiguousarray(xs[b])
        in_maps.append(m)
    res = run_bass_kernel_spmd(nc, in_maps, core_ids=list(range(8)))
    return np.stack([np.asarray(r["out"], np.float32) for r in res.results], axis=0)
```

```python
import sys
import contextlib
import math
import numpy as np
import ml_dtypes
import concourse.bass as bass
import concourse.mybir as mybir
from concourse.bass_utils import run_bass_kernel_spmd

F32 = mybir.dt.float32
BF16 = mybir.dt.bfloat16
AF = mybir.ActivationFunctionType
ALU = mybir.AluOpType
AX = mybir.AxisListType
NPBF = ml_dtypes.bfloat16

S = 2048
D = 1024
NT = 16
IN_COLS = 4888
NEGM = -30000.0
SCALE = 0.125
EPS = 1e-6
VC = 2063
LAMBDA_INIT = 0.2
DSZ = {F32: 4, BF16: 2}
BUCKET = 2048


def _dsize(dt):
    return DSZ.get(dt, 4)


def _region(ap):
    name = ap.tensor.name
    dims = ap.ap
    off = ap.offset
    dsz = _dsize(ap.dtype)
    space = str(ap.space)
    if space in ("SB", "PSUM"):
        pstep, pcnt = dims[0]
        p0 = ap.base_partition()
        f0 = off - p0 * pstep if pstep else off
        ext = 0
        for st, cnt in dims[1:]:
            ext += abs(st) * (cnt - 1)
            if st < 0:
                f0 += st * (cnt - 1)
        b0 = f0 * dsz
        b1 = (f0 + ext + 1) * dsz
        if space == "PSUM":
            b0 = (b0 // 2048) * 2048
            b1 = ((b1 + 2047) // 2048) * 2048
            return (name, 0, 128, b0, b1)
        return (name, p0, p0 + pcnt, b0, b1)
    lo = off
    hi = off
    for st, cnt in dims:
        if st >= 0:
            hi += st * (cnt - 1)
        else:
            lo += st * (cnt - 1)
    return (name, 0, 1, lo * dsz, (hi + 1) * dsz)


def _overlap(a, b):
    return a[1] < b[2] and b[1] < a[2] and a[3] < b[4] and b[3] < a[4]


def _covers(w, r):
    return w[1] <= r[1] and r[2] <= w[2] and w[3] <= r[3] and r[4] <= w[4]


class Op:
    __slots__ = ("eng", "emit", "reads", "writes", "deps", "is_dma", "dkey",
                 "idx", "need_inc", "src", "iname", "gall")


class Prog:
    def __init__(self, nc):
        self.nc = nc
        self.ops = []
        self.eng_obj = {"pe": nc.tensor, "dve": nc.vector, "act": nc.scalar,
                        "pool": nc.gpsimd, "sp": nc.sync}

    def add(self, eng, emit, reads=(), writes=(), dma=False, dkey=None, gall=False):
        o = Op()
        o.eng = eng
        o.emit = emit
        o.reads = [_region(a) for a in reads]
        o.writes = [_region(a) for a in writes]
        o.is_dma = dma
        o.dkey = dkey
        o.gall = gall
        o.idx = len(self.ops)
        o.need_inc = dma
        o.deps = ()
        f = sys._getframe(1)
        while f is not None and f.f_code.co_name in Prog.__dict__:
            f = f.f_back
        o.src = f.f_lineno if f is not None else -1
        o.iname = None
        self.ops.append(o)
        return o

    def mm(self, out, lhsT, rhs, start=True, stop=True):
        rd = [lhsT, rhs]
        if not start:
            rd.append(out)
        return self.add("pe", lambda e: e.matmul(out, lhsT, rhs, start=start, stop=stop),
                        rd, [out])

    def tr(self, out, in_, ident):
        return self.add("pe", lambda e: e.transpose(out, in_, ident), [in_, ident], [out])

    def dma(self, out, in_, q="sp", key=None, gall=False):
        if key is None:
            key = out.tensor.name
        return self.add(q, lambda e: e.dma_start(out=out, in_=in_), [in_], [out],
                        dma=True, dkey=key, gall=gall)

    def act(self, out, in_, func, bias=None, scale=None, accum_out=None):
        rd = [in_]
        wr = [out]
        kw = {}
        if bias is not None:
            kw["bias"] = bias
            if not isinstance(bias, (int, float)):
                rd.append(bias)
        if scale is not None:
            kw["scale"] = scale
            if not isinstance(scale, (int, float)):
                rd.append(scale)
        if accum_out is not None:
            kw["accum_out"] = accum_out
            wr.append(accum_out)
        return self.add("act", lambda e: e.activation(out, in_, func, **kw), rd, wr)

    def tt(self, eng, out, in0, in1, op):
        return self.add(eng, lambda e: e.tensor_tensor(out, in0, in1, op), [in0, in1], [out])

    def ts(self, eng, out, in0, s1, s2, op0, op1=None, accum_out=None):
        rd = [in0]
        wr = [out]
        for s in (s1, s2):
            if s is not None and not isinstance(s, (int, float)):
                rd.append(s)
        kw = {}
        if op1 is not None:
            kw["op1"] = op1
        if accum_out is not None:
            kw["accum_out"] = accum_out
            wr.append(accum_out)
        return self.add(eng, lambda e: e.tensor_scalar(out, in0, s1, s2, op0, **kw), rd, wr)

    def stt(self, eng, out, in0, scalar, in1, op0, op1):
        rd = [in0, in1]
        if not isinstance(scalar, (int, float)):
            rd.append(scalar)
        return self.add(eng, lambda e: e.scalar_tensor_tensor(out, in0, scalar, in1, op0, op1),
                        rd, [out])

    def copy(self, eng, out, in_):
        if eng == "act":
            return self.add("act", lambda e: e.copy(out, in_), [in_], [out])
        return self.add(eng, lambda e: e.tensor_copy(out, in_), [in_], [out])

    def memset(self, eng, ap, val):
        return self.add(eng, lambda e: e.memset(ap, val), [], [ap])

    def reduce(self, eng, out, in_, op):
        return self.add(eng, lambda e: e.tensor_reduce(out, in_, AX.X, op), [in_], [out])

    def recip(self, out, in_):
        return self.add("dve", lambda e: e.reciprocal(out, in_), [in_], [out])

    def finish(self, semf):
        ops = self.ops
        hist = {}

        def buckets(r):
            return range(r[3] // BUCKET, (r[4] - 1) // BUCKET + 1)

        for o in ops:
            deps = set()
            for r in o.reads:
                is_ps = (r[0] == "ps")
                for b in buckets(r):
                    for rec in hist.get((r[0], b), ()):
                        if _overlap(rec[0], r) and (rec[2] or (is_ps and ops[rec[1]].eng != o.eng)):
                            deps.add(rec[1])
            for w in o.writes:
                for b in buckets(w):
                    for rec in hist.get((w[0], b), ()):
                        if _overlap(rec[0], w):
                            deps.add(rec[1])
            deps.discard(o.idx)
            for w in o.writes:
                for b in buckets(w):
                    lst = hist.setdefault((w[0], b), [])
                    lst[:] = [rec for rec in lst if not _covers(w, rec[0])]
                    lst.append([w, o.idx, True])
            for r in o.reads:
                for b in buckets(r):
                    lst = hist.setdefault((r[0], b), [])
                    found = False
                    if not o.is_dma:
                        for rec in lst:
                            if (not rec[2]) and rec[0] == r:
                                po = ops[rec[1]]
                                if po.eng == o.eng and not po.is_dma:
                                    rec[1] = o.idx
                                    found = True
                                    break
                    if not found:
                        lst.append([r, o.idx, False])
            o.deps = deps
            for d in deps:
                ops[d].need_inc = True
        tl_cnt = {e: 0 for e in ("pe", "dve", "act", "pool")}
        tl_sem = {e: semf("tl_" + e) for e in ("pe", "dve", "act", "pool")}
        dma_sem = {}
        dma_cnt = {}
        token = [None] * len(ops)
        gall_ops = {}
        for o in ops:
            if o.is_dma:
                k = o.dkey
                if k not in dma_sem:
                    dma_sem[k] = semf("d_" + k)
                    dma_cnt[k] = 0
                dma_cnt[k] += 16
                token[o.idx] = [dma_sem[k], dma_cnt[k], "d_" + k]
                if o.gall:
                    gall_ops.setdefault((k, o.gall), []).append(o.idx)
            elif o.need_inc:
                tl_cnt[o.eng] += 1
                token[o.idx] = [tl_sem[o.eng], tl_cnt[o.eng], "tl_" + o.eng]
        for k, lst in gall_ops.items():
            mx = max(token[i][1] for i in lst)
            for i in lst:
                token[i][1] = mx
        seen = {e: {} for e in self.eng_obj}
        n_wait = 0
        for o in ops:
            e = self.eng_obj[o.eng]
            need = {}
            for d in o.deps:
                p = ops[d]
                if (not p.is_dma) and p.eng == "pe" and o.eng == "pe" and not o.is_dma:
                    continue
                sem, val, key = token[d]
                if need.get(key, (None, 0))[1] < val:
                    need[key] = (sem, val)
            for key, (sem, val) in need.items():
                if seen[o.eng].get(key, 0) >= val:
                    continue
                seen[o.eng][key] = val
                e.wait_ge(sem, val)
                n_wait += 1
            ins = o.emit(e)
            try:
                o.iname = ins.ins.name
            except Exception:
                pass
            tk = token[o.idx]
            if tk is not None:
                ins.then_inc(tk[0], 16 if o.is_dma else 1)
        self.final = {k: (dma_sem[k], dma_cnt[k]) for k in dma_sem}
        self.n_wait = n_wait
        self.imap = {o.iname: o.src for o in ops if o.iname}
        return self.final


def _t5_bucket_np(dist):
    n = np.maximum(dist, 0)
    nf = np.maximum(n, 1).astype(np.float32)
    large = 16 + (np.log(nf / np.float32(16)) / np.float32(math.log(8.0))
                  * np.float32(16)).astype(np.int32)
    large = np.minimum(large, 31)
    return np.where(n < 16, n, large)


def make_consts():
    c = {}
    eye = np.eye(128, dtype=np.float32)
    c["ident"] = eye.astype(NPBF)
    c["identf"] = eye
    c["jflip"] = eye[::-1].copy().astype(NPBF)
    j = np.arange(4096)
    dist = j - VC
    bk = _t5_bucket_np(dist)
    oh = np.zeros((33, 2, 4096), np.float32)
    for jj in range(4096):
        d = dist[jj]
        if d < 0:
            oh[32, 0, jj] = 1
            oh[32, 1, jj] = 1
        else:
            oh[bk[jj], 0, jj] = 1
            if d < 512:
                oh[bk[jj], 1, jj] = 1
            else:
                oh[32, 1, jj] = 1
    c["oh"] = oh.reshape(33, 8192).astype(NPBF)
    bsel = np.zeros((32, 2048), np.float32)
    for b in range(32):
        bsel[b, 64 * b:64 * b + 64] = 1
    c["bsel"] = bsel.astype(NPBF)
    q = np.arange(2048)
    cur = q // 64
    blk = np.arange(32)[None, :]
    valid = blk <= cur[:, None]
    forced = (blk == 0) | (blk == cur[:, None]) | (blk == cur[:, None] - 1)
    fc = np.where(valid, np.where(forced, 1e4, 0.0), -1e30).astype(np.float32)
    c["fc"] = fc.reshape(16, 128, 32).transpose(1, 0, 2).copy()
    n_cmp = 127
    c_start = np.arange(n_cmp) * 16
    s_start = np.arange(32) * 64
    lo = np.maximum(s_start[:, None], c_start[None, :])
    hi = np.minimum(s_start[:, None] + 64, c_start[None, :] + 32)
    ovl = (np.maximum(hi - lo, 0) / 32).astype(np.float32)
    ovlT = np.zeros((128, 32), np.float32)
    ovlT[:127] = ovl.T
    c["ovlT"] = ovlT.astype(NPBF)
    return c


class _Stop(Exception):
    pass


def build(nc, dbg=None, stop=None):
    dbg = dbg or set()
    P = Prog(nc)

    def ck(name):
        if stop == name:
            raise _Stop()
    dram_in = {}

    def din(name, shape, dt=F32):
        t = nc.dram_tensor(name, list(shape), dt, kind="ExternalInput").ap()
        dram_in[name] = t
        return t

    x = din("x", [S, D])
    g1T = din("g1T", [128, 8])
    w_in = din("w_in", [D, IN_COLS])
    gq_nsa = din("gq_nsa", [64, 1])
    gk_nsa = din("gk_nsa", [64, 3])
    posT = din("posT", [128, 2, 32])
    cmp_w1 = din("cmp_w1", [2, 2048, 256])
    cmp_w2 = din("cmp_w2", [2, 256, 64])
    gqd = din("gqd", [64, 2])
    gkd = din("gkd", [64, 2])
    dlam = din("dlam", [1, 256])
    gdo = din("gdo", [128, 1])
    rel_bias = din("rel_bias", [32, 12])
    w_bn = din("w_bn", [512, D])
    w_bd = din("w_bd", [512, D])
    w_out = din("w_out", [D, D])
    g2T = din("g2T", [128, 8])
    w_r = din("w_r", [D, 20])
    b_r = din("b_r", [1, 20])
    w_g = din("w_g", [16, D, 512])
    w_u = din("w_u", [16, D, 512])
    w_d = din("w_d", [16, 512, D])
    c_ident = din("ident", [128, 128], BF16)
    c_identf = din("identf", [128, 128], F32)
    c_jflip = din("jflip", [128, 128], BF16)
    c_oh = din("oh", [33, 8192], BF16)
    c_bsel = din("bsel", [32, 2048], BF16)
    c_fc = din("fc", [128, 16, 32], F32)
    c_ovlT = din("ovlT", [128, 32], BF16)
    out = nc.dram_tensor("out", [S, D], F32, kind="ExternalOutput").ap()
    vecs = nc.dram_tensor("vecs", [20, 4096], BF16).ap()
    dbg_outs = {}

    def dump(name, ap, dt=None):
        if name not in dbg:
            return
        shp = list(ap.shape)
        t = nc.dram_tensor("dbg_" + name, shp, dt or ap.dtype, kind="ExternalOutput").ap()
        dbg_outs[name] = t
        P.dma(t, ap, q="sp", key="out")

    ARENA_BYTES = 200 * 1024
    arena = nc.alloc_sbuf_tensor("arena", [128, ARENA_BYTES // 2], BF16)

    def A(off, shape, dt=BF16, parts=128):
        n = 1
        for s_ in shape:
            n *= s_
        nb = n * _dsize(dt)
        assert off % 4 == 0 and off + nb <= ARENA_BYTES, (off, shape)
        v = arena[0:parts, off // 2: (off + nb) // 2]
        if dt != BF16:
            v = v.bitcast(dt)
        if len(shape) == 2:
            v = v.rearrange("p (a b) -> p a b", a=shape[0])
        elif len(shape) == 3:
            v = v.rearrange("p (a b c) -> p a b c", a=shape[0], b=shape[1])
        elif len(shape) == 4:
            v = v.rearrange("p (a b c d) -> p a b c d", a=shape[0], b=shape[1], c=shape[2])
        return v

    cst = nc.alloc_sbuf_tensor("cst", [128, 1024], F32)
    cstb = nc.alloc_sbuf_tensor("cstb", [128, 512], BF16)
    ps = nc.alloc_psum_tensor("ps", [128, 4096], F32)

    def bank(b, n=512, parts=128):
        return ps[0:parts, b * 512: b * 512 + n]

    def bankbf(b, parts=128):
        return ps[0:parts, b * 512:(b + 1) * 512].bitcast(BF16)

    g1T_s = cst[:, 0:8]
    g2T_s = cst[:, 8:16]
    gq_s = cst[0:64, 16:17]
    gk_s = cst[0:64, 17:20]
    gqd_s = cst[0:64, 450:452]
    gkd_s = cst[0:64, 452:454]
    gdo_s = cst[:, 22:23]
    chcol = cst[:, 24:36]
    lamw = cst[:, 36:44]
    neglam = cst[:, 44:45]
    br_s = cst[:, 48:68]
    ss1 = cst[:, 72:88]
    rstd1 = cst[:, 88:104]
    ss2 = cst[:, 104:120]
    rstd2 = cst[:, 120:136]
    dl_s = cst[:, 136:392]
    tblx_f = cst[0:33, 392:404]
    pb_s = cst[:, 404:412]
    tiny = cst[:, 416:448]
    zl = cst[0:1, 512:576].bitcast(BF16)
    zr = cst[0:1, 576:736].bitcast(BF16)
    fc_s = nc.alloc_sbuf_tensor("fc_s", [128, 16, 32], F32)
    ident = cstb[:, 0:128]
    jflip = cstb[:, 128:256]
    ovlT_s = cstb[:, 256:288]
    tblx_b = cstb[0:33, 288:300]
    identf = nc.alloc_sbuf_tensor("identf_s", [128, 128], F32)

    O_HT = 0
    O_OT = 32768
    O_W = 65536
    WSLOT = 8 * 544 * 2
    O_N = O_W + 4 * WSLOT
    hT = A(O_HT, [8, 2048])
    oT = A(O_OT, [8, 2048])
    wslot = [A(O_W + i * WSLOT, [8, 544]) for i in range(4)]
    qT = A(O_N, [8, 2048])
    kslc = A(O_N + 32768, [2, 2048])
    kwin = A(O_N + 40960, [2, 2048])
    vnsa = A(O_N + 49152, [16, 2, 2, 65])
    kcv = A(O_N + 57600, [2, 2048])
    Tf = A(O_N + 65792, [12, 640])
    Tw = A(O_N + 81152, [8, 640])
    gates = A(O_N + 91392, [16, 24], F32)
    vcx = A(O_N + 92928, [2, 97])
    kcT = A(O_N + 93316, [2, 128])
    O_NEND = O_N + 93316 + 512
    xt = [A(O_N + i * 4096, [1024], F32) for i in range(2)]
    xn = [A(O_N + 8192 + i * 2048, [1024]) for i in range(2)]
    junk = A(O_N + 12288, [1024])
    o_acc = [A(O_W + i * 8192, [4, 512], F32) for i in range(2)]
    PT = [A(O_W + 16384 + i * 1024, [512]) for i in range(3)] + [A(O_W + 32768, [512])]
    Bc8 = A(O_W + 19456, [8, 512])
    ob = A(O_W + 27648, [4, 512])
    imp = A(O_W + 31744, [2, 4, 32], F32)
    O_S = 198656
    sm = A(O_S, [256], F32)
    qn_b = A(O_S + 1024, [512])
    sq_b = A(O_S + 2048, [512])
    sc2 = A(O_S + 3264, [64], F32)
    stage8 = A(O_S + 3520, [8, 96])
    qn_b2 = A(O_S + 5056, [512])
    dq = ["sp"]

    try:
        P.dma(g1T_s, g1T, key="setup0", gall=True)
        P.dma(g2T_s, g2T, key="setup0", gall=True)
        P.dma(gq_s, gq_nsa, key="setup0", gall=True)
        P.dma(gk_s, gk_nsa, key="setup0", gall=True)
        P.dma(gqd_s, gqd, key="setup0", gall=True)
        P.dma(gkd_s, gkd, key="setup0", gall=True)
        P.dma(gdo_s, gdo, key="setup0", gall=True)
        P.dma(chcol, bass.AP(rel_bias.tensor, 31 * 12, [[0, 128], [1, 12]]), key="setup0", gall=True)
        P.dma(br_s, bass.AP(b_r.tensor, 0, [[0, 128], [1, 20]]), key="setup0", gall=True)
        P.dma(dl_s, bass.AP(dlam.tensor, 0, [[0, 128], [1, 256]]), key="setup0", gall=True)
        P.dma(tblx_f[0:32, :], rel_bias, key="setup0", gall=True)
        P.dma(fc_s[:], c_fc, key="setup0", gall=True)
        P.dma(ident, c_ident, key="setup0", gall=True)
        P.dma(jflip, c_jflip, key="setup0", gall=True)
        P.dma(ovlT_s, c_ovlT, key="setup0", gall=True)
        P.dma(identf[:], c_identf, key="setup0", gall=True)
        P.dma(kslc[64:96, 0, :], c_bsel, key="setup0", gall=True)
        P.dma(kslc[64:96, 1, :], c_bsel, key="setup0", gall=True)
        oh_s = A(O_OT, [8192], parts=33)
        P.dma(oh_s, c_oh, key="setup0", gall=True)
        P.memset("dve", tblx_f[32:33, :], NEGM)
        P.memset("dve", tiny, 1e-30)
        P.memset("dve", cst[:, 512:1024], 0.0)
        P.copy("dve", tblx_b, tblx_f)
        P.ts("dve", gq_s, gq_s, SCALE, None, ALU.mult)
        P.ts("dve", gqd_s, gqd_s, SCALE, None, ALU.mult)
        P.ts("dve", gdo_s, gdo_s, 1.0 - LAMBDA_INIT, None, ALU.mult)
        P.tt("dve", sm[:, 0:64], dl_s[:, 0:64], dl_s[:, 64:128], ALU.mult)
        P.reduce("dve", lamw[:, 0:1], sm[:, 0:64], ALU.add)
        P.tt("dve", sm[:, 64:128], dl_s[:, 128:192], dl_s[:, 192:256], ALU.mult)
        P.reduce("dve", lamw[:, 1:2], sm[:, 64:128], ALU.add)
        P.act(lamw[:, 2:4], lamw[:, 0:2], AF.Exp)
        P.tt("dve", lamw[:, 4:5], lamw[:, 3:4], lamw[:, 2:3], ALU.subtract)
        P.ts("dve", neglam, lamw[:, 4:5], -LAMBDA_INIT, None, ALU.add)
        ck("setup")
        w_in_v = w_in.rearrange("(c p) n -> p c n", p=128)
        wctr = [0]

        def load_w(c0, n):
            i = wctr[0] % 4
            wctr[0] += 1
            P.dma(wslot[i][:, :, 0:n], w_in_v[:, :, c0:c0 + n], q="pool", key="w%d" % i)
            return wslot[i]

        wbA = load_w(0, 512)
        wbC = load_w(512, 256)
        def rms_stats(ssum_ap, rstd_ap, n):
            P.act(rstd_ap, ssum_ap, AF.Ln, scale=1.0 / n, bias=eps_col[0:rstd_ap.shape[0], :])
            P.act(rstd_ap, rstd_ap, AF.Exp, scale=-0.5)

        eps_col = cst[:, 448:449]
        P.memset("dve", eps_col, EPS)

        def skew(n, stage1, stage2, depth=1):
            for t_ in range(min(depth, n)):
                stage1(t_)
            for t_ in range(n):
                if t_ + depth < n:
                    stage1(t_ + depth)
                stage2(t_)

        def a_s1(t):
            sl = t % 2
            P.dma(xt[sl], x[128 * t:128 * t + 128, :], key="x%d" % sl)
            P.act(junk, xt[sl], AF.Square, accum_out=ss1[:, t:t + 1])
            rms_stats(ss1[:, t:t + 1], rstd1[:, t:t + 1], D)
            P.ts("dve", xn[sl], xt[sl], rstd1[:, t:t + 1], None, ALU.mult)

        def a_s2(t):
            sl = t % 2
            b0 = 2 + 2 * (t % 2)
            pst = ps[:, b0 * 512:(b0 + 2) * 512].rearrange("p (c k) -> p c k", c=8)
            for c in range(8):
                P.mm(pst[:, c, :], xn[sl][:, 128 * c:128 * c + 128], ident)
            for c in range(8):
                dst = hT[:, c, 128 * t:128 * t + 128]
                if c % 2 == 0:
                    P.ts("dve", dst, pst[:, c, :], g1T_s[:, c:c + 1], None, ALU.mult)
                else:
                    P.act(dst, pst[:, c, :], AF.Copy, scale=g1T_s[:, c:c + 1])
        skew(NT, a_s1, a_s2)
        dump("hT", hT)
        ck("A")
        vsb = A(O_OT + 16384, [4096], parts=12)
        vwb = A(O_OT + 24576, [4096], parts=8)
        for kind, nh, dst in ((0, 12, vsb), (1, 8, vwb)):
            for f in range(8):
                pb = bank(f % 2, 512, parts=nh)
                P.mm(pb, tblx_b[:, 0:nh], oh_s[:, kind * 4096 + f * 512: kind * 4096 + (f + 1) * 512])
                P.copy("dve" if f % 2 else "act", dst[:, f * 512:(f + 1) * 512], pb)
        P.dma(vecs[0:12, :], vsb, q="pool", key="vecs", gall=True)
        P.dma(vecs[12:20, :], vwb, q="pool", key="vecs", gall=True)
        for h in range(12):
            P.dma(Tf[:, h, :], bass.AP(vecs.tensor, h * 4096 + VC - 127, [[1, 128], [1, 640]]),
                  q="pool", key="toep", gall=True)
        for h in range(8):
            P.dma(Tw[:, h, :], bass.AP(vecs.tensor, (12 + h) * 4096 + VC - 127, [[1, 128], [1, 640]]),
                  q="pool", key="toep", gall=True)


        def proj_tok(t, wb, c0, n, pbank):
            for c in range(8):
                P.mm(pbank[:, 0:n], hT[:, c, 128 * t:128 * t + 128], wb[:, c, c0:c0 + n],
                     start=(c == 0), stop=(c == 7))

        def norm_heads(psb, nh, dst3, sel=None):
            P.act(sq_b[:, 0:nh * 64], psb, AF.Square)
            ssum = sm[:, 128:128 + nh]
            rstd = sm[:, 144:144 + nh]
            P.reduce("dve", ssum, sq_b[:, 0:nh * 64].rearrange("p (h d) -> p h d", d=64), ALU.add)
            rms_stats(ssum, rstd, 64)
            src = psb.rearrange("p (h d) -> p h d", d=64)
            r3 = rstd
            if sel is not None:
                src = src[:, sel, :]
                r3 = rstd[:, sel]
            k = src.shape[1]
            P.tt("dve", dst3, src, r3.unsqueeze(2).to_broadcast([128, k, 64]), ALU.mult)

        P.memset("pool", vnsa[:, :, :, :, 64:65], 1.0)

        wb = wbA
        wb_next = wbC
        qnb = [qn_b, qn_b2]

        def ba_s1(t, wb=wb):
            pb = bank(t % 3)
            proj_tok(t, wb, 0, 512, pb)
            norm_heads(pb, 8, qnb[t % 2].rearrange("p (h d) -> p h d", d=64))

        def ba_s2(t):
            b0 = 3 + 2 * (t % 2)
            pst = ps[0:64, b0 * 512:(b0 + 2) * 512].rearrange("p (h k) -> p h k", h=8)
            for h in range(8):
                P.mm(pst[:, h, :], qnb[t % 2][:, 64 * h:64 * h + 64], ident)
            P.ts("dve", qT[0:64, :, 128 * t:128 * t + 128], pst, gq_s, None, ALU.mult)
        skew(NT, ba_s1, ba_s2)
        wb = wb_next
        wb_next = load_w(768, 536)
        for m in range(2):
            for Q in range(4):
                pb = bank((m * 4 + Q) % 2)
                for c in range(8):
                    P.mm(pb, wb[:, c, 128 * m:128 * m + 128], hT[:, c, 512 * Q:512 * Q + 512],
                         start=(c == 0), stop=(c == 7))
                P.copy("act" if Q % 2 else "dve", kcv[:, m, 512 * Q:512 * Q + 512], pb)
        wb = wb_next
        gate_ps = bank(7, 384).rearrange("p (t g) -> p t g", g=24)

        def bb_s1(t, wb=wb):
            pb = bank(t % 3)
            proj_tok(t, wb, 0, 512, pb)
            for c in range(8):
                P.mm(gate_ps[:, t, :], hT[:, c, 128 * t:128 * t + 128], wb[:, c, 512:536],
                     start=(c == 0), stop=(c == 7))
            kn = qnb[t % 2][:, 0:256].rearrange("p (h d) -> p h d", d=64)
            P.act(sq_b, pb, AF.Square)
            ssum = sm[:, 128:136]
            rstd = sm[:, 144:152]
            P.reduce("dve", ssum, sq_b.rearrange("p (h d) -> p h d", d=64), ALU.add)
            rms_stats(ssum, rstd, 64)
            p3 = pb.rearrange("p (h d) -> p h d", d=64)
            for br in range(2):
                P.tt("dve", kn[:, 2 * br:2 * br + 2, :], p3[:, 4 * br:4 * br + 2, :],
                     rstd[:, 4 * br:4 * br + 2].unsqueeze(2).to_broadcast([128, 2, 64]), ALU.mult)
                P.copy("dve", vnsa[:, t, br, :, 0:64], p3[:, 4 * br + 2:4 * br + 4, :])

        def bb_s2(t):
            pst = bank(3 + 2 * (t % 2), parts=64).rearrange("p (h k) -> p h k", h=4)
            for j in range(4):
                P.mm(pst[:, j, :], qnb[t % 2][:, 64 * j:64 * j + 64], ident)
            P.ts("dve", kslc[0:64, :, 128 * t:128 * t + 128], pst[:, 0:2, :], gk_s[:, 1:2], None, ALU.mult)
            P.ts("dve", kwin[0:64, :, 128 * t:128 * t + 128], pst[:, 2:4, :], gk_s[:, 2:3], None, ALU.mult)
        skew(NT, bb_s1, bb_s2)
        gflat = gates.rearrange("p t g -> p (t g)")
        P.act(gflat, bank(7, 384), AF.Exp, scale=-1.0)
        P.ts("dve", gflat, gflat, 1.0, None, ALU.add)
        P.recip(gflat, gflat)
        dump("qT", qT)
        dump("kslc", kslc)
        dump("kwin", kwin)
        dump("vnsa", vnsa)
        dump("gates", gates)

        ck("B")
        w1s = [A(O_W + i * 16384, [32, 256]) for i in range(2)]
        w2s = A(O_OT, [2, 2, 64])
        zs = A(O_OT + 1024, [8, 128], F32)
        z2 = A(O_OT + 5120, [8, 128], F32)
        hidT = A(O_OT + 9216, [8, 128])
        for m in range(2):
            src = cmp_w1[m].rearrange("(l d) c -> d l c", d=64)
            for l0 in range(0, 32, 8):
                P.dma(w1s[m][0:64, l0:l0 + 8, :], src[:, l0:l0 + 8, :], q="pool", key="w1_%d" % m, gall=True)
            P.dma(w2s[:, m, :, :], cmp_w2[m].rearrange("(cc p) d -> p cc d", p=128), q="pool", key="w2s", gall=True)
        posf = A(O_OT + 11776, [64], F32)
        P.dma(posf, posT.rearrange("p m l -> p (m l)"), key="posb")
        tokl = A(O_OT + 12288, [32, 128], parts=64)
        kcv1 = A(O_OT + 20480, [2, 2048], parts=64)
        for m in range(2):
            P.dma(kcv1[:, m, :], kcv[64:128, m, :], key="kcv1", gall=True)
        P.memset("pool", tokl, 0.0)
        ck("c1")
        hps = ps[:, 6 * 512:8 * 512].rearrange("p (i n) -> p i n", n=128)
        for m in range(2):
            for g in range(2):
                srcg = kcv[0:64] if g == 0 else kcv1
                for l in range(32):
                    P.ts("dve", tokl[:, l, 0:127], srcg[:, m, l:l + 16 * 126 + 1:16],
                         posf[0:64, m * 32 + l:m * 32 + l + 1], None, ALU.add)
                for cc in range(2):
                    i = (m * 2 + g) * 2 + cc
                    for l in range(32):
                        P.mm(hps[:, i, :], w1s[m][0:64, l, 128 * cc:128 * cc + 128],
                             tokl[:, l, :], start=(l == 0), stop=(l == 31))
        for i2 in range(2):
            P.copy("act", zs[:, 4 * i2:4 * i2 + 4, :], hps[:, 4 * i2:4 * i2 + 4, :])
        ck("c2")
        zf = zs.rearrange("p i n -> p (i n)")
        z2f = z2.rearrange("p i n -> p (i n)")
        P.tt("dve", z2f, zf, zf, ALU.mult)
        P.ts("dve", z2f, z2f, 0.044715, 1.0, ALU.mult, ALU.add)
        P.tt("dve", z2f, z2f, zf, ALU.mult)
        P.act(z2f, z2f, AF.Exp, scale=-2.0 * math.sqrt(2.0 / math.pi))
        P.ts("dve", z2f, z2f, 1.0, None, ALU.add)
        P.recip(z2f, z2f)
        P.tt("dve", hidT.rearrange("p i n -> p (i n)"), z2f, zf, ALU.mult)
        ck("c3")
        cps = bank(5, 256).rearrange("p (i d) -> p i d", d=64)
        for m in range(2):
            for g in range(2):
                for cc in range(2):
                    i = (m * 2 + g) * 2 + cc
                    P.mm(cps[:, m * 2 + g, :], hidT[:, i, :], w2s[:, m, cc, :],
                         start=(cc == 0), stop=(cc == 1))
        P.act(sq_b[:, 0:128], cps[:, 0:2, :].rearrange("p i d -> p (i d)"), AF.Square)
        P.reduce("dve", sm[:, 128:130], sq_b[:, 0:128].rearrange("p (h d) -> p h d", d=64), ALU.add)
        rms_stats(sm[:, 128:130], sm[:, 144:146], 64)
        kcn = qn_b[:, 0:128].rearrange("p (h d) -> p h d", d=64)
        P.tt("dve", kcn, cps[:, 0:2, :], sm[:, 144:146].unsqueeze(2).to_broadcast([128, 2, 64]), ALU.mult)
        P.copy("act", vcx[:, :, 0:64], cps[:, 2:4, :])
        P.memset("dve", vcx[:, :, 64:65], 1.0)
        for g in range(2):
            P.copy("dve", vcx[:, g, 65:97], ovlT_s)
        pst = bank(3, parts=64).rearrange("p (h k) -> p h k", h=4)
        for g in range(2):
            P.mm(pst[:, g, :], qn_b[:, 64 * g:64 * g + 64], ident)
        P.ts("dve", kcT[0:64, :, :], pst[:, 0:2, :], gk_s[:, 0:1], None, ALU.mult)
        dump("kcT", kcT)
        dump("vcx", vcx)

        ck("cmp")
        P.memset("dve", stage8[:, :, 0:64], 0.0)

        pcfg = {"sb": [0, 1, 2]}

        def sbank(k):
            return bank(pcfg["sb"][k % len(pcfg["sb"])])

        def run_items(items, depth=2):
            n = len(items)
            deferred = []

            def defer(k, delay, fn):
                deferred.append([k + delay, fn])

            def flush(k):
                rest = []
                for d_ in list(deferred):
                    if d_[0] <= k:
                        d_[1]()
                    else:
                        rest.append(d_)
                deferred[:] = rest

            def issueS(k):
                it = items[k]
                if it.get("pre"):
                    it["pre"]()
                it["S"](k)
            for k in range(min(depth, n)):
                issueS(k)
            for k in range(n):
                flush(k)
                items[k]["E"](k)
                if k + depth < n:
                    issueS(k + depth)
                items[k]["V"](k)
                if items[k].get("post"):
                    items[k]["post"](k, defer)
            flush(10 ** 9)

        def attn_items(items, Q, kT_ap, q_ap, Ttab, th, vfun, vw, accf, jlo_fun, use_T_always,
                       pre_first=None, post_last=None, pts=None):
            pts = pts or PT
            i0 = 4 * Q
            started = set()
            last_of_bank = {}
            for tt_ in range(4):
                last_of_bank[_region(accf(tt_))[3] // 2048] = tt_
            jmin = min(jlo_fun(i) for i in range(i0, i0 + 4))
            first = len(items)
            for j in range(jmin, i0 + 4):
                tiles = [i for i in range(i0, i0 + 4) if jlo_fun(i) <= j <= i]
                i_lo, i_hi = tiles[0], tiles[-1]
                n = (i_hi - i_lo + 1) * 128
                c0 = (i_lo - i0) * 128
                need_T = use_T_always or (j >= i0 - 1)
                x0 = (i_lo - j) * 128

                def fS(k, j=j, n=n, c0=c0, need_T=need_T, x0=x0):
                    sb = sbank(k)
                    P.mm(sb[:, 0:n], kT_ap(j), q_ap(c0, n), start=True, stop=not need_T)
                    if need_T:
                        P.mm(sb[:, 0:n], jflip, Ttab[:, th, x0:x0 + n], start=False, stop=True)

                def fE(k, n=n, need_T=need_T):
                    sb = sbank(k)
                    pt = pts[k % len(pts)]
                    if need_T:
                        P.act(pt[:, 0:n], sb[:, 0:n], AF.Exp)
                    else:
                        P.act(pt[:, 0:n], sb[:, 0:n], AF.Exp, bias=chcol[:, th:th + 1])

                def fV(k, j=j, tiles=tiles, i_lo=i_lo):
                    pt = pts[k % len(pts)]
                    for i in tiles:
                        oap = accf(i - i0)
                        bk_ = _region(oap)[3] // 2048
                        P.mm(oap, pt[:, (i - i_lo) * 128:(i - i_lo) * 128 + 128], vfun(j),
                             start=(bk_ not in started), stop=(j == i and i - i0 == last_of_bank[bk_]))
                        started.add(bk_)
                items.append(dict(S=fS, E=fE, V=fV))
            items[first]["pre"] = pre_first
            items[-1]["post"] = post_last

        def finalize_nsa(acc, Q, h, br):
            i0 = 4 * Q
            oa = o_acc[Q % 2]
            rs = sm[:, 160:164]
            P.ts("dve", rs, acc[:, :, 64], 1e-30, None, ALU.max)
            P.recip(rs, rs)
            P.tt("dve", rs, rs, gates[:, i0:i0 + 4, br * 8 + h], ALU.mult)
            for tt_ in range(4):
                dst = oa[:, tt_, 64 * h:64 * h + 64]
                P.stt("dve", dst, acc[:, tt_, 0:64], rs[:, tt_:tt_ + 1], dst, ALU.mult, ALU.add)

        acc_rot = [5, 6, 7]
        accc = [0]

        def next_acc():
            b_ = acc_rot[accc[0] % len(acc_rot)]
            accc[0] += 1
            return b_

        def load_bc(Q):
            for h in range(8):
                P.dma(Bc8[:, h, :], bass.AP(vecs.tensor, h * 4096 + 512 * Q, [[16, 128], [1, 512]]),
                      key="bc8", gall="q%d" % Q)

        def sel_dve(Q):
            i0 = 4 * Q
            for g in range(2):
                for tt_ in range(4):
                    t = i0 + tt_
                    sc = sc2[:, 0:32]
                    m8 = sc2[:, 32:48]
                    scr = sm[:, 192:224]
                    P.tt("dve", sc, imp[:, g, tt_, :], fc_s[:, t, :], ALU.add)
                    P.add("dve", lambda e, m8=m8, sc=sc: e.max(out=m8[:, 0:8], in_=sc), [sc], [m8[:, 0:8]])
                    P.add("dve", lambda e, m8=m8, sc=sc, scr=scr: e.match_replace(
                        out=scr, in_to_replace=m8[:, 0:8], in_values=sc, imm_value=-3e38),
                        [m8[:, 0:8], sc], [scr])
                    P.add("dve", lambda e, m8=m8, scr=scr: e.max(out=m8[:, 8:16], in_=scr), [scr], [m8[:, 8:16]])
                    P.ts("dve", stage8[:, g * 4 + tt_, 64:96], sc, m8[:, 15:16], NEGM, ALU.is_lt, ALU.mult)

        def sel_pe(Q):
            i0 = 4 * Q
            for g in range(2):
                pst = bank(3, 512, parts=96)
                for tt_ in range(4):
                    P.mm(pst[:, 128 * tt_:128 * tt_ + 128], stage8[:, g * 4 + tt_, 0:96], ident)
                P.copy("act", qT[64:96, 4 * g:4 * g + 4, 512 * Q:512 * Q + 512],
                       pst[64:96, :].unsqueeze(1).to_broadcast([32, 4, 512]))

        def conv_a(Q):
            for tt_ in range(4):
                P.copy("dve", ob[:, tt_, :], o_acc[Q % 2][:, tt_, :])

        def conv_b(Q):
            for tt_ in range(4):
                t = 4 * Q + tt_
                pst = bank(3).rearrange("p (c k) -> p c k", c=4)
                for k_ in range(4):
                    P.mm(pst[:, k_, :], ob[:, tt_, 128 * k_:128 * k_ + 128], ident)
                P.copy("dve", oT[:, 0:4, 128 * t:128 * t + 128], pst[:, 0:4, :])

        items = []
        load_bc(0)
        for Q in range(4):
            i0 = 4 * Q
            qs = slice(512 * Q, 512 * Q + 512)
            for h in range(8):
                g = h // 4
                acc = bank(next_acc(), 4 * 97).rearrange("p (t v) -> p t v", v=97)

                def fS(k, g=g, h=h, qs=qs):
                    sb = sbank(k)
                    P.mm(sb, kcT[0:64, g, :], qT[0:64, h, qs], start=True, stop=False)
                    P.mm(sb, jflip, Bc8[:, h, :], start=False, stop=True)

                def fE(k):
                    P.act(PT[k % len(PT)], sbank(k), AF.Exp)

                def fV(k, acc=acc, g=g):
                    for tt_ in range(4):
                        P.mm(acc[:, tt_, :], PT[k % len(PT)][:, 128 * tt_:128 * tt_ + 128], vcx[:, g, :])

                def post(k, defer, acc=acc, g=g, h=h, Q=Q, i0=i0):
                    rs = sm[:, 160:164]
                    P.ts("dve", rs, acc[:, :, 64], 1e-30, None, ALU.max)
                    P.recip(rs, rs)
                    for tt_ in range(4):
                        dsti = imp[:, g, tt_, :]
                        if h % 4 == 0:
                            P.ts("dve", dsti, acc[:, tt_, 65:97], rs[:, tt_:tt_ + 1], None, ALU.mult)
                        else:
                            P.stt("dve", dsti, acc[:, tt_, 65:97], rs[:, tt_:tt_ + 1], dsti, ALU.mult, ALU.add)
                    P.tt("dve", rs, rs, gates[:, i0:i0 + 4, h], ALU.mult)
                    for tt_ in range(4):
                        P.ts("dve", o_acc[Q % 2][:, tt_, 64 * h:64 * h + 64], acc[:, tt_, 0:64],
                             rs[:, tt_:tt_ + 1], None, ALU.mult)
                    if h == 7:
                        sel_dve(Q)
                        if Q + 1 < 4:
                            load_bc(Q + 1)
                items.append(dict(S=fS, E=fE, V=fV, post=post))
            for h in range(8):
                g = h // 4
                acc = bank(next_acc(), 4 * 65).rearrange("p (t v) -> p t v", v=65)
                attn_items(items, Q,
                           lambda j, g=g: kwin[0:64, g, 128 * j:128 * j + 128],
                           lambda c0, n, h=h, Q=Q: qT[0:64, h, 512 * Q + c0:512 * Q + c0 + n],
                           Tw, h, lambda j, g=g: vnsa[:, j, 1, g, :], 65,
                           lambda tt_, acc=acc: acc[:, tt_, 0:65],
                           lambda i: max(0, i - 4), True,
                           post_last=lambda k, defer, acc=acc, Q=Q, h=h: finalize_nsa(acc, Q, h, 2))
            for h in range(8):
                g = h // 4
                acc = bank(next_acc(), 4 * 65).rearrange("p (t v) -> p t v", v=65)
                if h == 7:
                    def post_l(k, defer, acc=acc, Q=Q, h=h):
                        finalize_nsa(acc, Q, h, 1)
                        defer(k, 2, lambda Q=Q: conv_a(Q))
                        defer(k, 6, lambda Q=Q: conv_b(Q))
                else:
                    def post_l(k, defer, acc=acc, Q=Q, h=h):
                        finalize_nsa(acc, Q, h, 1)
                attn_items(items, Q,
                           lambda j, g=g: kslc[0:96, g, 128 * j:128 * j + 128],
                           lambda c0, n, h=h, Q=Q: qT[0:96, h, 512 * Q + c0:512 * Q + c0 + n],
                           Tf, h, lambda j, g=g: vnsa[:, j, 0, g, :], 65,
                           lambda tt_, acc=acc: acc[:, tt_, 0:65],
                           lambda i: 0, False,
                           pre_first=(lambda Q=Q: sel_pe(Q)) if h == 0 else None,
                           post_last=post_l)
        pcfg["sb"] = [0, 1, 2, 4]
        run_items(items, depth=3)
        pcfg["sb"] = [0, 1, 2]
        dump("qTm", qT)
        dump("oTn", oT)

        ck("nsa")
        qd = A(O_N, [8, 2048])
        kd = A(O_N + 32768, [8, 2048])
        vd = A(O_N + 81152, [16, 4, 129])
        P.memset("pool", vd[:, :, :, 128:129], 1.0)
        wb = load_w(1304, 512)
        wb_next = load_w(1816, 512)
        for blk in range(3):
            if blk < 2:
                dstT = qd if blk == 0 else kd
                gsc = gqd_s if blk == 0 else gkd_s

                def d_s1(t, wb=wb):
                    pb = bank(t % 3)
                    proj_tok(t, wb, 0, 512, pb)
                    norm_heads(pb, 8, qnb[t % 2].rearrange("p (h d) -> p h d", d=64))

                def d_s2(t, dstT=dstT, gsc=gsc):
                    b0 = 3 + 2 * (t % 2)
                    pst = ps[0:64, b0 * 512:(b0 + 2) * 512].rearrange("p (h k) -> p h k", h=8)
                    for hh in range(8):
                        P.mm(pst[:, hh, :], qnb[t % 2][:, 64 * hh:64 * hh + 64], ident)
                    for i_ in range(2):
                        P.ts("dve", dstT[0:64, i_::2, 128 * t:128 * t + 128], pst[:, i_::2, :],
                             gsc[:, i_:i_ + 1], None, ALU.mult)
                skew(NT, d_s1, d_s2)
            else:
                for t in range(NT):
                    pb = bank(t % 3)
                    proj_tok(t, wb, 0, 512, pb)
                    P.copy("act" if t % 2 else "dve", vd[:, t, :, 0:128], pb.rearrange("p (h d) -> p h d", d=128))
            if blk == 0:
                wb = wb_next
                wb_next = load_w(2328, 512)
            elif blk == 1:
                wb = wb_next
        dump("qd", qd)
        dump("kd", kd)

        O_DT = O_N + 65792
        PTd = [A(O_DT + i * 1024, [512]) for i in range(3)]
        o1s = [A(O_DT + 3072 + i * 2048, [4, 128], F32) for i in range(2)]
        obd = [A(O_DT + 7168 + i * 1024, [4, 128]) for i in range(2)]
        GM0 = 2840
        blocks = {}
        P.dma(wslot[0][:, :, 0:512], w_in_v[:, :, GM0:GM0 + 512], q="pool", key="w0")
        P.dma(wslot[1][:, :, 0:512], w_in_v[:, :, GM0 + 1024:GM0 + 1536], q="pool", key="w1")
        blocks[(0, 0)] = wslot[0]
        blocks[(1, 0)] = wslot[1]
        wbn = A(O_W + 17408, [4, 1024])
        wbd = A(O_W + 25600, [4, 1024])
        P.dma(wbn, w_bn.rearrange("(k p) n -> p k n", p=128), q="pool", key="wbn")
        P.dma(wbd, w_bd.rearrange("(k p) n -> p k n", p=128), q="pool", key="wbd")
        items = []
        acc_sets = [(5, 6), (4, 7)]
        dcnt = [0]
        for Q in range(4):
            for h in range(4):
                hb = (Q * 4 + h) % 2
                for i_ in range(2):
                    aset = acc_sets[dcnt[0] % 2]
                    dcnt[0] += 1
                    accv = [ps[:, aset[tt_ // 2] * 512 + (tt_ % 2) * 160:aset[tt_ // 2] * 512 + (tt_ % 2) * 160 + 129]
                            for tt_ in range(4)]

                    def stage_a(Q=Q, h=h, hb=hb):
                        for tt_ in range(4):
                            P.act(sq_b[:, 0:128], o1s[hb][:, tt_, :], AF.Square,
                                  accum_out=sm[:, 168 + tt_:169 + tt_])
                        rms_stats(sm[:, 168:172], sm[:, 172:176], 128)
                        for tt_ in range(4):
                            P.ts("dve", obd[hb][:, tt_, :], o1s[hb][:, tt_, :], sm[:, 172 + tt_:173 + tt_],
                                 None, ALU.mult)

                    def stage_b(Q=Q, h=h, hb=hb):
                        pst = bank(3).rearrange("p (c k) -> p c k", c=4)
                        for tt_ in range(4):
                            P.mm(pst[:, tt_, :], obd[hb][:, tt_, :], ident)
                        P.act(oT[:, 4 + h, 512 * Q:512 * Q + 512], pst[:, 0:4, :].rearrange("p c k -> p (c k)"),
                              AF.Copy, scale=gdo_s)

                    def post(k, defer, accv=accv, i_=i_, hb=hb, stage_a=stage_a, stage_b=stage_b):
                        rs = sm[:, 160:164]
                        for tt_ in range(4):
                            P.ts("dve", rs[:, tt_:tt_ + 1], accv[tt_][:, 128:129], 1e-30, None, ALU.max)
                        P.recip(rs, rs)
                        if i_ == 0:
                            for tt_ in range(4):
                                P.ts("dve", o1s[hb][:, tt_, :], accv[tt_][:, 0:128], rs[:, tt_:tt_ + 1],
                                     None, ALU.mult)
                        else:
                            P.ts("dve", rs, rs, neglam[:, 0:1], None, ALU.mult)
                            for tt_ in range(4):
                                P.stt("dve", o1s[hb][:, tt_, :], accv[tt_][:, 0:128], rs[:, tt_:tt_ + 1],
                                      o1s[hb][:, tt_, :], ALU.mult, ALU.add)
                            defer(k, 5, stage_a)
                            defer(k, 10, stage_b)
                    attn_items(items, Q,
                               lambda j, h=h, i_=i_: kd[0:64, 2 * h + i_, 128 * j:128 * j + 128],
                               lambda c0, n, h=h, i_=i_, Q=Q: qd[0:64, 2 * h + i_, 512 * Q + c0:512 * Q + c0 + n],
                               Tf, 8 + h, lambda j, h=h: vd[:, j, h, :], 129,
                               lambda tt_, accv=accv: accv[tt_],
                               lambda i: 0, False, post_last=post, pts=PTd)
        run_items(items)
        dump("oT", oT)

        ck("diff")
        yT = A(O_N, [8, 2048])
        gsc_ = [A(O_N + 49152 + 2048 + i * 2048, [512], F32) for i in range(4)]
        ysc = [A(O_N + 59392 + i * 2048, [512], F32) for i in range(2)]
        gmB = [A(O_N + 32768 + i * WSLOT, [8, 544]) for i in range(2)]
        P.dma(gmB[0][:, :, 0:512], w_in_v[:, :, GM0 + 512:GM0 + 1024], q="pool", key="gmb0")
        P.dma(gmB[1][:, :, 0:512], w_in_v[:, :, GM0 + 1536:GM0 + 2048], q="pool", key="gmb1")
        blocks[(0, 1)] = gmB[0]
        blocks[(1, 1)] = gmB[1]
        wo_v = w_out.rearrange("(c p) n -> p c n", p=128)
        it = 0
        for cc in range(8):
            hb = cc // 4
            co = 128 * (cc % 4)
            if cc == 4:
                for hf in range(2):
                    P.dma(wslot[hf][:, :, 0:512], wo_v[:, :, 512 * hf:512 * hf + 512], q="pool", key="w%d" % hf)
            for Q in range(4):
                qs = slice(512 * Q, 512 * Q + 512)
                pg = [bank(0 + 2 * (it % 2)), bank(1 + 2 * (it % 2))]
                pbm = [bank(4 + 2 * (it % 2)), bank(5 + 2 * (it % 2))]
                for s_ in range(2):
                    wbk = blocks[(s_, hb)]
                    for c in range(8):
                        P.mm(pg[s_], wbk[:, c, co:co + 128], hT[:, c, qs], start=(c == 0), stop=(c == 7))
                for k in range(4):
                    P.mm(pbm[0], wbn[:, k, 128 * cc:128 * cc + 128], oT[:, k, qs], start=(k == 0), stop=(k == 3))
                for k in range(4):
                    P.mm(pbm[1], wbd[:, k, 128 * cc:128 * cc + 128], oT[:, 4 + k, qs], start=(k == 0), stop=(k == 3))
                e0 = gsc_[2 * (it % 2)]
                e1 = gsc_[2 * (it % 2) + 1]
                P.act(e0, pg[0], AF.Sigmoid)
                P.act(e1, pg[1], AF.Sigmoid)
                y0 = ysc[it % 2]
                P.tt("dve", y0, pbm[0], e0, ALU.mult)
                P.tt("dve", e1, pbm[1], e1, ALU.mult)
                P.tt("dve", yT[:, cc, qs], y0, e1, ALU.add)
                it += 1
        dump("yT", yT)

        ck("g1")
        x1 = A(O_N + 32768, [16, 1024], F32)
        h2T = A(O_HT, [8, 2048])
        wo = [wslot[0], wslot[1]]
        xt2 = [A(O_OT + i * 4096, [1024], F32) for i in range(2)]
        xhi = [A(O_OT + 8192 + i * 4096, [1024]) for i in range(2)]
        xlo = [A(O_OT + 8192 + i * 4096 + 2048, [1024]) for i in range(2)]
        hiT = [A(O_OT + 16384 + i * 4096, [8, 128]) for i in range(2)]
        loT = [A(O_OT + 16384 + i * 4096 + 2048, [8, 128]) for i in range(2)]
        whi = A(O_OT + 25216, [8, 20])
        wlo = A(O_OT + 28672, [8, 20])
        wr_s = A(O_OT + 24576, [8, 20], F32)
        junk2 = A(O_OT + 25600, [1024])
        comb = A(O_OT + 27648, [16, 16], F32)
        rsm = A(O_OT + 28672, [128], F32)
        P.dma(wr_s, w_r.rearrange("(c p) n -> p c n", p=128), key="wr")
        P.tt("dve", wr_s, wr_s, g2T_s[:, 0:8].unsqueeze(2).to_broadcast([128, 8, 20]), ALU.mult)
        P.copy("dve", whi, wr_s)
        P.tt("dve", wlo, wr_s, whi, ALU.subtract)
        lgall = A(O_OT + 29184, [16, 20], F32)
        mw = [(A(O_W + 17408, [8, 512]), A(O_W + 25600, [8, 512]), A(O_W, [4, 1024])),
              (A(O_N, [8, 512]), A(O_N + 8192, [8, 512]), A(O_N + 16384, [4, 1024]))]
        hid = [A(O_N + 24576 + i * 4096, [4, 512]) for i in range(2)]
        sg = [A(O_W + 8192 + i * 1024, [512]) for i in range(2)]
        it = 0

        def load_expert(e, parts=(0, 1, 2)):
            wg_, wu_, wd_ = mw[e % 2]
            k = "moe%d" % (e % 2)
            if 0 in parts:
                P.dma(wg_, w_g[e].rearrange("(c p) n -> p c n", p=128), q="pool", key=k, gall="e%d" % e)
            if 1 in parts:
                P.dma(wu_, w_u[e].rearrange("(c p) n -> p c n", p=128), q="pool", key=k, gall="e%d" % e)
            if 2 in parts:
                P.dma(wd_, w_d[e].rearrange("(c p) n -> p c n", p=128), q="pool", key="moed%d" % (e % 2))
        load_expert(0, parts=(0, 1))

        def g2_s1(t):
            sl = t % 2
            P.dma(xt2[sl], x[128 * t:128 * t + 128, :], key="x%d" % sl)
            for hf in range(2):
                pb = bank(hf)
                for cc in range(8):
                    P.mm(pb, yT[:, cc, 128 * t:128 * t + 128], wo[hf][:, cc, 0:512],
                         start=(cc == 0), stop=(cc == 7))
                P.tt("dve", x1[:, t, 512 * hf:512 * hf + 512], pb, xt2[sl][:, 512 * hf:512 * hf + 512], ALU.add)
            P.act(junk2, x1[:, t, :], AF.Square, accum_out=ss2[:, t:t + 1])
            rms_stats(ss2[:, t:t + 1], rstd2[:, t:t + 1], D)
            P.act(xhi[sl], x1[:, t, :], AF.Copy, scale=rstd2[:, t:t + 1])
            P.stt("dve", xlo[sl], x1[:, t, :], rstd2[:, t:t + 1], xhi[sl], ALU.mult, ALU.subtract)

        def g2_s2(t):
            sl = t % 2
            for src, dstT, b0 in ((xhi[sl], hiT[sl], 2), (xlo[sl], loT[sl], 4)):
                for half in range(2):
                    pstf = bank(b0 + half).rearrange("p (c k) -> p c k", c=4)
                    for c4 in range(4):
                        c = half * 4 + c4
                        P.mm(pstf[:, c4, :], src[:, 128 * c:128 * c + 128], ident)
                    P.copy("act" if half else "dve", dstT[:, 4 * half:4 * half + 4, :], pstf)
            P.tt("dve", h2T[:, :, 128 * t:128 * t + 128], hiT[sl],
                 g2T_s[:, 0:8].unsqueeze(2).to_broadcast([128, 8, 128]), ALU.mult)
            pr = ps[:, 6 * 512 + 32 * (t % 2):6 * 512 + 32 * (t % 2) + 20]
            k_ = 0
            for aT, w_ in ((hiT[sl], whi), (hiT[sl], wlo), (loT[sl], whi)):
                for c in range(8):
                    P.mm(pr, aT[:, c, :], w_[:, c, :], start=(k_ == 0), stop=(k_ == 23))
                    k_ += 1
            P.tt("dve", lgall[:, t, :], pr, br_s, ALU.add)
        skew(NT, g2_s1, g2_s2)
        R = A(O_OT + 30464, [576], F32)
        gl = lgall[:, :, 0:4]
        el = lgall[:, :, 4:20].rearrange("p t (g e) -> p t g e", e=4)
        gmax = R[:, 0:16]
        P.reduce("dve", gmax, gl, ALU.max)
        gsh = R[:, 16:80].rearrange("p (t g) -> p t g", g=4)
        P.tt("dve", gsh, gl, gmax.unsqueeze(2).to_broadcast([128, 16, 4]), ALU.subtract)
        goh = R[:, 80:144].rearrange("p (t g) -> p t g", g=4)
        P.tt("dve", goh, gl, gmax.unsqueeze(2).to_broadcast([128, 16, 4]), ALU.is_ge)
        P.act(R[:, 16:80], R[:, 16:80], AF.Exp)
        gsum = R[:, 144:160]
        P.reduce("dve", gsum, gsh, ALU.add)
        P.recip(gsum, gsum)
        msk = R[:, 160:416].rearrange("p (t g e) -> p t g e", g=4, e=4)
        P.tt("dve", msk, el, goh.unsqueeze(3).to_broadcast([128, 16, 4, 4]), ALU.mult)
        ein = R[:, 416:480].rearrange("p (t e) -> p t e", e=4)
        P.reduce("dve", ein, msk.rearrange("p t g e -> p t e g"), ALU.add)
        m1 = R[:, 480:496]
        P.reduce("dve", m1, ein, ALU.max)
        eq1 = R[:, 496:560].rearrange("p (t e) -> p t e", e=4)
        P.tt("dve", eq1, ein, m1.unsqueeze(2).to_broadcast([128, 16, 4]), ALU.is_ge)
        ein2 = R[:, 160:224].rearrange("p (t e) -> p t e", e=4)
        P.stt("dve", ein2, eq1, -1e30, ein, ALU.mult, ALU.add)
        m2 = R[:, 560:576]
        P.reduce("dve", m2, ein2, ALU.max)
        mask2 = R[:, 224:288].rearrange("p (t e) -> p t e", e=4)
        P.tt("dve", mask2, ein, m2.unsqueeze(2).to_broadcast([128, 16, 4]), ALU.is_ge)
        esh = R[:, 288:352].rearrange("p (t e) -> p t e", e=4)
        P.tt("dve", esh, ein, m1.unsqueeze(2).to_broadcast([128, 16, 4]), ALU.subtract)
        P.act(R[:, 288:352], R[:, 288:352], AF.Exp)
        P.tt("dve", esh, esh, mask2, ALU.mult)
        den = R[:, 352:368]
        P.reduce("dve", den, esh, ALU.add)
        P.recip(den, den)
        P.tt("dve", den, den, gsum, ALU.mult)
        P.tt("dve", esh, esh, den.unsqueeze(2).to_broadcast([128, 16, 4]), ALU.mult)
        P.tt("dve", comb.rearrange("p t (g e) -> p t g e", e=4),
             goh.unsqueeze(3).to_broadcast([128, 16, 4, 4]),
             esh.unsqueeze(2).to_broadcast([128, 16, 4, 4]), ALU.mult)
        dump("x1", x1)
        dump("comb", comb)
        dump("h2T", h2T)
        ck("g2")
        load_expert(0, parts=(2,))

        for e in range(16):
            if e + 1 < 16:
                load_expert(e + 1)
            elif False:
                pass
            wg_, wu_, wd_ = mw[e % 2]
            for Q in range(4):
                qs = slice(512 * Q, 512 * Q + 512)
                hb = hid[it % 2]
                for fc in range(4):
                    pgb = bank(0 + (fc % 2) * 2)
                    pub = bank(1 + (fc % 2) * 2)
                    for c in range(8):
                        P.mm(pgb, wg_[:, c, 128 * fc:128 * fc + 128], h2T[:, c, qs], start=(c == 0), stop=(c == 7))
                    for c in range(8):
                        P.mm(pub, wu_[:, c, 128 * fc:128 * fc + 128], h2T[:, c, qs], start=(c == 0), stop=(c == 7))
                    sgb = sg[fc % 2]
                    P.act(sgb, pgb, AF.Silu)
                    P.tt("dve", hb[:, fc, :], sgb, pub, ALU.mult)
                for tt_ in range(4):
                    t = 4 * Q + tt_
                    for hf in range(2):
                        po = bank(4 + (2 * tt_ + hf) % 4)
                        for fc in range(4):
                            P.mm(po, hb[:, fc, 128 * tt_:128 * tt_ + 128], wd_[:, fc, 512 * hf:512 * hf + 512],
                                 start=(fc == 0), stop=(fc == 3))
                        dst = x1[:, t, 512 * hf:512 * hf + 512]
                        P.stt("dve", dst, po, comb[:, t, e:e + 1], dst, ALU.mult, ALU.add)
                    if e == 15:
                        P.dma(out[128 * t:128 * t + 128, :], x1[:, t, :], key="out")
                it += 1

    except _Stop:
        pass
    es = contextlib.ExitStack()

    def semf(name):
        return es.enter_context(nc.semaphore(name))
    fin = P.finish(semf)
    for k, (s_, v) in fin.items():
        nc.sync.wait_ge(s_, v)
    es.close()
    return P, dram_in, dbg_outs


def prep_shared(inputs):
    f = lambda a: np.ascontiguousarray(np.asarray(a, dtype=np.float32))
    d = {}
    d["g1T"] = f(inputs["norm1_g"][0].reshape(8, 128).T)
    d["w_in"] = f(inputs["w_in"][0])
    d["gq_nsa"] = f(inputs["nsa_q_norm"][0].reshape(64, 1))
    d["gk_nsa"] = f(inputs["nsa_k_norm"][0].T)
    pos = np.asarray(inputs["cmp_pos"][0], np.float32)
    pT = np.transpose(pos, (2, 0, 1))
    d["posT"] = f(np.concatenate([pT, pT], axis=0))
    d["cmp_w1"] = f(inputs["cmp_w1"][0])
    d["cmp_w2"] = f(inputs["cmp_w2"][0])
    d["gqd"] = f(inputs["diff_q_norm"][0].T)
    d["gkd"] = f(inputs["diff_k_norm"][0].T)
    d["dlam"] = f(inputs["diff_lambda"][0].reshape(1, 256))
    d["gdo"] = f(inputs["diff_out_norm"][0].reshape(128, 1))
    d["rel_bias"] = f(inputs["rel_bias"])
    d["w_bn"] = f(inputs["w_branch_nsa"][0])
    d["w_bd"] = f(inputs["w_branch_diff"][0])
    d["w_out"] = f(inputs["w_out"][0])
    d["g2T"] = f(inputs["norm2_g"][0].reshape(8, 128).T)
    d["w_r"] = f(np.concatenate([inputs["router_group_w"][0], inputs["router_expert_w"][0]], axis=1))
    d["b_r"] = f(np.concatenate([inputs["router_group_b"][0], inputs["router_expert_b"][0]]).reshape(1, 20))
    d["w_g"] = f(inputs["expert_w_gate"][0])
    d["w_u"] = f(inputs["expert_w_up"][0])
    d["w_d"] = f(inputs["expert_w_down"][0])
    d.update(make_consts())
    return d


def kernel(**inputs):
    nc = bass.Bass("TRN2", target_bir_lowering=False)
    build(nc)
    shared = prep_shared(inputs)
    xs = np.asarray(inputs["x"], np.float32)
    in_maps = []
    for b in range(8):
        m = dict(shared)
        m["x"] = np.ascontiguousarray(xs[b])
        in_maps.append(m)
    res = run_bass_kernel_spmd(nc, in_maps, core_ids=list(range(8)))
    return np.stack([np.asarray(r["out"], np.float32) for r in res.results], axis=0)
```
